# Optimizing a Trainium2 kernel written in Bass

```python
import math
import jax, jax.numpy as jnp
from jax import lax
import numpy as np

D_MODEL = 1024
BATCH = 16
SEQ = 4096
DEPTH = 2
DEC_BATCH = 8
DEC_SEQ = 4096
PAST_LEN = 128

N_META = 16
GRID_W = 64
N_HEADS = 8
N_KV_HEADS = 2
HEAD_DIM = 64
ATTN_WIDTH = N_HEADS * HEAD_DIM
KV_WIDTH = N_KV_HEADS * HEAD_DIM
ATTN_IN = ATTN_WIDTH + 2 * KV_WIDTH
Q_BLOCK = 128
ROPE_THETA = 10000.0
ROPE_PAIRS = HEAD_DIM // 2
ROPE_PAIRS_PER_AXIS = ROPE_PAIRS // 2
HYENA_WIDTH = D_MODEL - ATTN_WIDTH
HY_GROUPS = 8
HY_GROUP_DIM = HYENA_WIDTH // HY_GROUPS
HY_SHORT = 3
HY_BANDS = 16
HY_EMB = 1 + 2 * HY_BANDS
HY_FH = 64
IN_WIDTH = ATTN_IN + 3 * HYENA_WIDTH
PEER_HEADS = 8
PEER_NKEYS = 128
PEER_EXPERTS = PEER_NKEYS * PEER_NKEYS
PEER_TOPK = 16
PEER_DKEY = 256
PEER_DHALF = PEER_DKEY // 2
PEER_CHUNK = 256
NORM_EPS = 1e-6

kernel_name = "hymba_attn_hyena_peer_encoder"


def _rmsnorm(x, g):
    xf = x.astype(jnp.float32)
    y = xf * lax.rsqrt(jnp.mean(xf * xf, axis=-1, keepdims=True) + NORM_EPS)
    return (y * g.astype(jnp.float32)).astype(x.dtype)


def _group_rmsnorm(x, g):
    shp = x.shape
    return _rmsnorm(x.reshape(shp[:-1] + g.shape), g).reshape(shp)


def _axial_rope(n_tok):
    rows = n_tok // GRID_W
    row = jnp.repeat(jnp.arange(rows, dtype=jnp.float32), GRID_W)
    col = jnp.tile(jnp.arange(GRID_W, dtype=jnp.float32), rows)
    inv = ROPE_THETA ** (-jnp.arange(ROPE_PAIRS_PER_AXIS, dtype=jnp.float32) / ROPE_PAIRS_PER_AXIS)
    ang = jnp.concatenate([row[:, None] * inv, col[:, None] * inv], axis=-1)
    ang = jnp.concatenate([jnp.zeros((N_META, ROPE_PAIRS), jnp.float32), ang], axis=0)
    return jnp.cos(ang), jnp.sin(ang)


def _apply_rope(x, cos, sin):
    xr = x.reshape(x.shape[:-1] + (ROPE_PAIRS, 2))
    x0, x1 = xr[..., 0], xr[..., 1]
    c = cos[None, :, None, :]
    s = sin[None, :, None, :]
    return jnp.stack([x0 * c - x1 * s, x0 * s + x1 * c], axis=-1).reshape(x.shape)


def _attention_branch(a_in, q_norm_g, k_norm_g, cos, sin):
    B, L, _ = a_in.shape
    dt = a_in.dtype
    q, k, v = jnp.split(a_in, [ATTN_WIDTH, ATTN_WIDTH + KV_WIDTH], axis=-1)
    q = q.reshape(B, L, N_HEADS, HEAD_DIM).astype(jnp.float32)
    k = k.reshape(B, L, N_KV_HEADS, HEAD_DIM).astype(jnp.float32)
    v = v.reshape(B, L, N_KV_HEADS, HEAD_DIM)
    q = _apply_rope(_rmsnorm(q, q_norm_g), cos, sin).astype(dt)
    k = _apply_rope(_rmsnorm(k, k_norm_g), cos, sin).astype(dt)
    G = N_HEADS // N_KV_HEADS
    scale = HEAD_DIM ** -0.5
    qg = q.reshape(B, L, N_KV_HEADS, G, HEAD_DIM)

    def block(qb):
        s = jnp.einsum('bqkgd,bskd->bkgqs', qb, k, preferred_element_type=jnp.float32) * scale
        p = jax.nn.softmax(s, axis=-1)
        return jnp.einsum('bkgqs,bskd->bqkgd', p.astype(v.dtype), v)

    o_meta = block(qg[:, :N_META]).reshape(B, N_META, ATTN_WIDTH)
    S = L - N_META
    nb = S // Q_BLOCK
    qr = qg[:, N_META:].reshape(B, nb, Q_BLOCK, N_KV_HEADS, G, HEAD_DIM).transpose(1, 0, 2, 3, 4, 5)
    o_real = lax.map(block, qr)
    o_real = o_real.transpose(1, 0, 2, 3, 4, 5).reshape(B, S, ATTN_WIDTH)
    return jnp.concatenate([o_meta, o_real], axis=1)


def _hyena_filter(L, w1, b1, sin_freq, w2, b2, w3, decay):
    f32 = jnp.float32
    t = jnp.arange(L, dtype=f32)
    tn = t / (L - 1)
    bands = jnp.linspace(1e-4, HY_BANDS - 1, HY_BANDS, dtype=f32)
    w = (2.0 * math.pi / L) * t
    z = jnp.concatenate([tn[:, None], jnp.cos(w[:, None] * bands), jnp.sin(w[:, None] * bands)], axis=-1)
    sf = sin_freq.astype(f32)
    h = jnp.sin(sf[0] * (z @ w1.astype(f32) + b1.astype(f32)))
    h = jnp.sin(sf[1] * (h @ w2.astype(f32) + b2.astype(f32)))
    h = (h @ w3.astype(f32)).reshape(L, 2, HYENA_WIDTH)
    h = h * jnp.exp(-tn[:, None, None] * decay.astype(f32)[None])
    hf, hb = h[:, 0], h[:, 1]
    return jnp.concatenate([hf, jnp.zeros((1, HYENA_WIDTH), f32), hb[:0:-1]], axis=0)


def _hyena_branch(z_in, conv_w, conv_b, filt, dskip):
    dt = z_in.dtype
    zp = jnp.pad(z_in, ((0, 0), (1, 1), (0, 0)))
    z = zp[:, :-2] * conv_w[0] + zp[:, 1:-1] * conv_w[1] + zp[:, 2:] * conv_w[2] + conv_b
    x0, x1, vv = jnp.split(z, 3, axis=-1)
    u = (vv * x0).astype(jnp.float32)
    L = u.shape[1]
    U = jnp.fft.rfft(u, n=2 * L, axis=1)
    Hf = jnp.fft.rfft(filt, axis=0)
    y = jnp.fft.irfft(U * Hf[None], n=2 * L, axis=1)[:, :L]
    y = y + u * dskip.astype(jnp.float32)
    return (y * x1.astype(jnp.float32)).astype(dt)


def _peer(x, w_q, sub_keys, u_tab, v_tab):
    T = x.shape[0]
    pad = (-T) % PEER_CHUNK
    xc = jnp.pad(x, ((0, pad), (0, 0))).reshape(-1, PEER_CHUNK, D_MODEL)

    def chunk(xb):
        q = (xb @ w_q).reshape(PEER_CHUNK, PEER_HEADS, 2, PEER_DHALF)
        s = jnp.einsum('chpd,phnd->chpn', q, sub_keys, preferred_element_type=jnp.float32)
        s1, i1 = lax.top_k(s[:, :, 0], PEER_TOPK)
        s2, i2 = lax.top_k(s[:, :, 1], PEER_TOPK)
        cand = (s1[..., :, None] + s2[..., None, :]).reshape(PEER_CHUNK, PEER_HEADS, PEER_TOPK * PEER_TOPK)
        cidx = (i1[..., :, None] * PEER_NKEYS + i2[..., None, :]).reshape(PEER_CHUNK, PEER_HEADS, PEER_TOPK * PEER_TOPK)
        top_s, top_j = lax.top_k(cand, PEER_TOPK)
        eidx = jnp.take_along_axis(cidx, top_j, axis=-1)
        g = jax.nn.softmax(top_s, axis=-1)
        ue = jnp.take(u_tab, eidx, axis=0)
        ve = jnp.take(v_tab, eidx, axis=0)
        a = jax.nn.gelu(jnp.einsum('chkd,cd->chk', ue, xb, preferred_element_type=jnp.float32), approximate=False)
        return jnp.einsum('chk,chkd->cd', (g * a).astype(xb.dtype), ve)

    out = lax.map(chunk, xc).reshape(-1, D_MODEL)
    return out[:T]


def _encode(x, meta_tokens, norm1_g, w_in, q_norm_g, k_norm_g, hy_conv_w, hy_conv_b,
            hy_ffn_w1, hy_ffn_b1, hy_sin_freq, hy_ffn_w2, hy_ffn_b2, hy_ffn_w3, hy_decay, hy_dskip,
            attn_out_g, hy_out_g, w_out, norm2_g, peer_wq, peer_keys, peer_u, peer_v, final_g):
    B, S, _ = x.shape
    L = S + N_META
    dt = x.dtype
    h = jnp.concatenate([jnp.broadcast_to(meta_tokens.astype(dt)[None], (B, N_META, D_MODEL)), x], axis=1)
    cos, sin = _axial_rope(S)
    for l in range(DEPTH):
        hn = _rmsnorm(h, norm1_g[l])
        proj = hn @ w_in[l]
        a_in, z_in = proj[..., :ATTN_IN], proj[..., ATTN_IN:]
        o_att = _attention_branch(a_in, q_norm_g[l], k_norm_g[l], cos, sin)
        filt = _hyena_filter(L, hy_ffn_w1[l], hy_ffn_b1[l], hy_sin_freq[l], hy_ffn_w2[l],
                             hy_ffn_b2[l], hy_ffn_w3[l], hy_decay[l])
        o_hy = _hyena_branch(z_in, hy_conv_w[l], hy_conv_b[l], filt, hy_dskip[l])
        mixed = jnp.concatenate([_group_rmsnorm(o_att, attn_out_g[l]),
                                 _group_rmsnorm(o_hy, hy_out_g[l])], axis=-1)
        h = h + mixed @ w_out[l]
        hn = _rmsnorm(h, norm2_g[l])
        h = h + _peer(hn.reshape(B * L, D_MODEL), peer_wq[l], peer_keys[l], peer_u[l], peer_v[l]).reshape(B, L, D_MODEL)
    h = _rmsnorm(h, final_g)
    return h[:, N_META:]


def setup_inputs(seed: int = 0) -> dict:
    key = jax.random.key(seed)
    ks = jax.random.split(key, 32)
    f32 = jnp.float32
    nrm = lambda k, shp, sc: jax.random.normal(k, shp, f32) * sc
    gain = lambda k, shp: 1.0 + 0.02 * jax.random.normal(k, shp, f32)
    return {
        "x_prompt": nrm(ks[0], (BATCH, SEQ, D_MODEL), 1.0),
        "x_sample": nrm(ks[1], (DEC_BATCH, DEC_SEQ, D_MODEL), 1.0),
        "meta_tokens": nrm(ks[2], (N_META, D_MODEL), 1.0),
        "norm1_g": gain(ks[3], (DEPTH, D_MODEL)),
        "w_in": nrm(ks[4], (DEPTH, D_MODEL, IN_WIDTH), D_MODEL ** -0.5),
        "q_norm_g": gain(ks[5], (DEPTH, HEAD_DIM)),
        "k_norm_g": gain(ks[6], (DEPTH, HEAD_DIM)),
        "hy_conv_w": nrm(ks[7], (DEPTH, HY_SHORT, 3 * HYENA_WIDTH), HY_SHORT ** -0.5),
        "hy_conv_b": nrm(ks[8], (DEPTH, 3 * HYENA_WIDTH), 0.02),
        "hy_ffn_w1": nrm(ks[9], (DEPTH, HY_EMB, HY_FH), HY_EMB ** -0.5),
        "hy_ffn_b1": nrm(ks[10], (DEPTH, HY_FH), 0.02),
        "hy_sin_freq": gain(ks[11], (DEPTH, 2, HY_FH)),
        "hy_ffn_w2": nrm(ks[12], (DEPTH, HY_FH, HY_FH), HY_FH ** -0.5),
        "hy_ffn_b2": nrm(ks[13], (DEPTH, HY_FH), 0.02),
        "hy_ffn_w3": nrm(ks[14], (DEPTH, HY_FH, 2 * HYENA_WIDTH), 0.1 * HY_FH ** -0.5),
        "hy_decay": jax.random.uniform(ks[15], (DEPTH, 2, HYENA_WIDTH), f32, 3.0, 15.0),
        "hy_dskip": nrm(ks[16], (DEPTH, HYENA_WIDTH), 1.0),
        "attn_out_g": gain(ks[17], (DEPTH, N_HEADS, HEAD_DIM)),
        "hy_out_g": gain(ks[18], (DEPTH, HY_GROUPS, HY_GROUP_DIM)),
        "w_out": nrm(ks[19], (DEPTH, D_MODEL, D_MODEL), 0.5 * D_MODEL ** -0.5),
        "norm2_g": gain(ks[20], (DEPTH, D_MODEL)),
        "peer_wq": nrm(ks[21], (DEPTH, D_MODEL, PEER_HEADS * PEER_DKEY), D_MODEL ** -0.5),
        "peer_keys": nrm(ks[22], (DEPTH, 2, PEER_HEADS, PEER_NKEYS, PEER_DHALF), PEER_DHALF ** -0.5),
        "peer_u": nrm(ks[23], (DEPTH, PEER_EXPERTS, D_MODEL), D_MODEL ** -0.5),
        "peer_v": nrm(ks[24], (DEPTH, PEER_EXPERTS, D_MODEL), 0.5 * PEER_HEADS ** -0.5),
        "final_g": gain(ks[25], (D_MODEL,)),
    }


def reference(x_prompt, x_sample, meta_tokens, norm1_g, w_in, q_norm_g, k_norm_g, hy_conv_w, hy_conv_b,
              hy_ffn_w1, hy_ffn_b1, hy_sin_freq, hy_ffn_w2, hy_ffn_b2, hy_ffn_w3, hy_decay, hy_dskip,
              attn_out_g, hy_out_g, w_out, norm2_g, peer_wq, peer_keys, peer_u, peer_v, final_g):
    y_prompt = _encode(x_prompt, meta_tokens, norm1_g, w_in, q_norm_g, k_norm_g, hy_conv_w, hy_conv_b,
                       hy_ffn_w1, hy_ffn_b1, hy_sin_freq, hy_ffn_w2, hy_ffn_b2, hy_ffn_w3, hy_decay, hy_dskip,
                       attn_out_g, hy_out_g, w_out, norm2_g, peer_wq, peer_keys, peer_u, peer_v, final_g)
    y_sample = _encode(x_sample, meta_tokens, norm1_g, w_in, q_norm_g, k_norm_g, hy_conv_w, hy_conv_b,
                       hy_ffn_w1, hy_ffn_b1, hy_sin_freq, hy_ffn_w2, hy_ffn_b2, hy_ffn_w3, hy_decay, hy_dskip,
                       attn_out_g, hy_out_g, w_out, norm2_g, peer_wq, peer_keys, peer_u, peer_v, final_g)
    return (y_prompt, y_sample)
```

```python
import math
from contextlib import ExitStack
import numpy as np
import ml_dtypes
import concourse.bass as bass
import concourse.mybir as mybir
from concourse.bass_utils import run_bass_kernel_spmd

F32 = mybir.dt.float32
BF16 = mybir.dt.bfloat16
U8 = mybir.dt.uint8
U32 = mybir.dt.uint32
I32 = mybir.dt.int32
ALU = mybir.AluOpType
AF = mybir.ActivationFunctionType
AX = mybir.AxisListType

D = 1024
NMETA = 16
DEPTH = 2
NEXP = 16384
EPS = 1e-6
TWO_PI = 2.0 * math.pi


class Trk:
    __slots__ = ("w", "r")

    def __init__(self):
        self.w = None
        self.r = {}


class _Rec:
    def __getattr__(self, name):
        def f(*a, **k):
            return (name, a, k)
        return f


_REC = _Rec()


class Prog:
    ENG = ("pe", "dve", "act", "pool", "sp")

    def __init__(self, nc, es):
        self.nc = nc
        self.ops = {e: [] for e in self.ENG}
        self.sems = []
        self.val = []

        def newsem(name):
            h = es.enter_context(nc.semaphore(name))
            self.sems.append(h)
            self.val.append(0)
            return len(self.sems) - 1

        self.csem = {e: newsem("c_" + e) for e in ("pe", "dve", "act", "pool")}
        self.dsem = {"sp": [newsem("d_sp%d" % i) for i in range(12)],
                     "pool": [newsem("d_pl%d" % i) for i in range(8)],
                     "act": [newsem("d_ac%d" % i) for i in range(6)]}
        self.dn = {"sp": 0, "pool": 0, "act": 0}
        self.known = {e: {} for e in self.ENG}

    def _waits(self, eng, toks):
        kn = self.known[eng]
        for (s, v) in toks:
            if eng == "pe" and s == self.csem["pe"]:
                continue
            if kn.get(s, 0) < v:
                kn[s] = v
                self.ops[eng].append(("w", self.sems[s], v))

    @staticmethod
    def _deps(reads, writes):
        toks = []
        for t in reads:
            if t.w is not None:
                toks.append(t.w)
        for t in writes:
            if t.w is not None:
                toks.append(t.w)
            toks.extend(t.r.items())
        return toks

    @staticmethod
    def _mark(tok, reads, writes):
        for t in writes:
            t.w = tok
            t.r = {}
        for t in reads:
            if t in writes:
                continue
            if t.r.get(tok[0], 0) < tok[1]:
                t.r[tok[0]] = tok[1]

    def op(self, eng, fn, reads=(), writes=(), inc=True):
        self._waits(eng, self._deps(reads, writes))
        s = self.csem[eng]
        if inc:
            self.val[s] += 1
            tok = (s, self.val[s])
            self.ops[eng].append(("i", fn(_REC), self.sems[s], 1))
        else:
            tok = (s, self.val[s] + 1)
            self.ops[eng].append(("n", fn(_REC)))
        self._mark(tok, reads, writes)

    def dma(self, q, fn, reads=(), writes=()):
        toks = self._deps(reads, writes)
        pool = self.dsem[q]
        s = pool[self.dn[q] % len(pool)]
        self.dn[q] += 1
        if self.val[s] > 0:
            toks.append((s, self.val[s]))
        self._waits(q, toks)
        self.val[s] += 16
        tok = (s, self.val[s])
        self.ops[q].append(("i", fn(_REC), self.sems[s], 16))
        self._mark(tok, reads, writes)

    def barrier(self):
        allt = [(s, v) for s, v in enumerate(self.val) if v > 0]
        for e in self.ENG:
            self._waits(e, allt)

    def replay(self, eng, e):
        for it in self.ops[eng]:
            if it[0] == "w":
                e.wait_ge(it[1], it[2])
            elif it[0] == "i":
                c = it[1]
                getattr(e, c[0])(*c[1], **c[2]).then_inc(it[2], it[3])
            else:
                c = it[1]
                getattr(e, c[0])(*c[1], **c[2])


class _Stop(Exception):
    pass


import os
_KSTOP = os.environ.get("KSTOP", "")


def _chk(tag):
    if _KSTOP and _KSTOP == tag:
        raise _Stop()


def mk(base, dims, parts=None, p0=0, off=0):
    ps = list(base.ap[0])
    np_ = ps[1] if parts is None else parts
    return bass.AP(tensor=base.tensor, offset=base.offset + p0 * ps[0] + off,
                   ap=[[ps[0], np_]] + [[int(s), int(c)] for s, c in dims])


def build(S, NSEQ):
    L = S + NMETA
    NT = (L + 127) // 128
    LP = NT * 128
    LR = L - (NT - 1) * 128
    assert LP > L
    nc = bass.Bass("TRN2", target_bir_lowering=False)
    es = ExitStack()

    def din(name, shape, dt=F32):
        return nc.dram_tensor(name, list(shape), dt, kind="ExternalInput").ap()

    def dscr(name, shape, dt=F32):
        return nc.dram_tensor(name, list(shape), dt, kind="Internal").ap()

    x = din("x", [NSEQ, S, D])
    meta = din("meta_tokens", [NMETA, D])
    norm1_g = din("norm1_g", [DEPTH, D]); w_in = din("w_in", [DEPTH, D, 2304])
    q_norm_g = din("q_norm_g", [DEPTH, 64]); k_norm_g = din("k_norm_g", [DEPTH, 64])
    hy_conv_w = din("hy_conv_w", [DEPTH, 3, 1536]); hy_conv_b = din("hy_conv_b", [DEPTH, 1536])
    hy_ffn_w1 = din("hy_ffn_w1", [DEPTH, 33, 64]); hy_ffn_b1 = din("hy_ffn_b1", [DEPTH, 64])
    hy_sin_freq = din("hy_sin_freq", [DEPTH, 2, 64]); hy_ffn_w2 = din("hy_ffn_w2", [DEPTH, 64, 64])
    hy_ffn_b2 = din("hy_ffn_b2", [DEPTH, 64]); hy_ffn_w3 = din("hy_ffn_w3", [DEPTH, 64, 1024])
    hy_decay = din("hy_decay", [DEPTH, 2, 512]); hy_dskip = din("hy_dskip", [DEPTH, 512])
    attn_out_g = din("attn_out_g", [DEPTH, 8, 64]); hy_out_g = din("hy_out_g", [DEPTH, 8, 64])
    w_out = din("w_out", [DEPTH, D, D]); norm2_g = din("norm2_g", [DEPTH, D])
    peer_wq = din("peer_wq", [DEPTH, D, 2048]); peer_keys = din("peer_keys", [DEPTH, 2, 8, 128, 128])
    peer_u = din("peer_u", [DEPTH, NEXP, D]); peer_v = din("peer_v", [DEPTH, NEXP, D])
    final_g = din("final_g", [D])
    c_cos = din("c_cos", [LP, 32]); c_sin = din("c_sin", [LP, 32])
    c_C = din("c_C", [NT, 128, NT, 128], BF16); c_S = din("c_S", [NT, 128, NT, 128], BF16)
    c_zfT = din("c_zfT", [33, LP]); c_col = din("c_col", [128, 3, NT])
    c_idb = din("c_idb", [128, 128], BF16); c_idf = din("c_idf", [128, 128])
    c_iota = din("c_iota", [128, 16])
    c_io128 = din("c_io128", [128, 128], BF16)
    yout = nc.dram_tensor("y", [NSEQ, S, D], F32, kind="ExternalOutput").ap()
    H = dscr("H", [NSEQ, LP, D])
    Zs = dscr("Zs", [NSEQ, LP + 2, 1536])
    X1s = dscr("X1s", [NSEQ, LP, 512])
    HRI = dscr("HRI", [NT, 128, 2, 512])
    MIXA = dscr("MIXA", [NSEQ, 64, 8, LP], BF16)
    MIXH = dscr("MIXH", [NSEQ, 128, 4, LP], BF16)
    H2s = dscr("H2s", [NSEQ, LP, D])
    HN2T = dscr("HN2T", [NSEQ, 128, 8, LP], BF16)
    RT = dscr("RT", [NSEQ, NT, 128, 384])
    UTs = dscr("UTs", [128, 128, 1024], BF16)
    Vs = dscr("Vs", [128, 128, 1024], BF16)

    ARENA = 200 * 1024
    arena = es.enter_context(nc.sbuf_tensor("arena", [128, ARENA], U8))
    psum = [es.enter_context(nc.psum_tensor("ps%d" % i, [128, 512], F32)) for i in range(8)]
    P = Prog(nc, es)

    class Alloc:
        def __init__(self, start):
            self.off = start

        def get(self, nelem, dt, parts=128):
            bs = {F32: 4, BF16: 2, U32: 4, U8: 1, I32: 4}[dt]
            nb = (nelem * bs + 31) // 32 * 32
            o = self.off
            self.off += nb
            assert self.off <= ARENA, ("arena overflow", self.off)
            return arena[0:parts, o:o + nelem * bs].bitcast(dt)

    PS = [p[:] for p in psum]
    PSB = [p[:].bitcast(BF16) for p in psum]
    PT_ = [Trk() for _ in range(8)]

    def col(j):
        return slice(j * 128, (j + 1) * 128)

    A0 = Alloc(0)
    idb = A0.get(128, BF16); idf = A0.get(128, F32)
    ccol = A0.get(3 * NT, F32)
    iota16 = A0.get(16, F32)
    negpi = A0.get(1, F32)
    epsc = A0.get(1, F32)
    Tc = Trk()
    P.dma("sp", lambda e: e.dma_start(out=idb, in_=c_idb), writes=[Tc])
    P.dma("sp", lambda e: e.dma_start(out=idf, in_=c_idf), writes=[Tc])
    P.dma("sp", lambda e: e.dma_start(out=ccol, in_=c_col.rearrange("p a n -> p (a n)")), writes=[Tc])
    P.dma("sp", lambda e: e.dma_start(out=iota16, in_=c_iota), writes=[Tc])
    P.op("dve", lambda e: e.memset(negpi, -math.pi), writes=[Tc])
    P.op("dve", lambda e: e.memset(epsc, EPS), writes=[Tc])

    def rsqrt_(dst, src, t, scale, use_eps=True, extra_reads=()):
        if use_eps:
            P.op("act", lambda e: e.activation(out=dst, in_=src, func=AF.Sqrt, scale=scale, bias=epsc[0:dst.shape[0], 0:1]),
                 reads=[t, Tc] + list(extra_reads), writes=[t])
        else:
            P.op("act", lambda e: e.activation(out=dst, in_=src, func=AF.Sqrt, scale=scale), reads=[t] + list(extra_reads), writes=[t])
        P.op("dve", lambda e: e.reciprocal(out=dst, in_=dst), reads=[t], writes=[t])
    negtn = lambda j: ccol[:, j:j + 1]
    valid = lambda j: ccol[:, NT + j:NT + j + 1]
    wfc = lambda j: ccol[:, 2 * NT + j:2 * NT + j + 1]
    COMMON_END = A0.off
    P.barrier()

    TH = {}

    def th(*key):
        if key not in TH:
            TH[key] = Trk()
        return TH[key]

    def load_h(layer, s, i, dst, tdst, q="sp"):
        if layer > 0:
            P.dma(q, lambda e: e.dma_start(out=dst, in_=H[s, i * 128:(i + 1) * 128, :]),
                  reads=[th("H", s, i)], writes=[tdst])
            return
        if i == 0:
            P.dma(q, lambda e: e.dma_start(out=dst[0:NMETA, :], in_=meta), writes=[tdst])
            P.dma(q, lambda e: e.dma_start(out=dst[NMETA:128, :], in_=x[s, 0:128 - NMETA, :]), writes=[tdst])
        elif i < NT - 1:
            P.dma(q, lambda e: e.dma_start(out=dst, in_=x[s, i * 128 - NMETA:i * 128 + 128 - NMETA, :]), writes=[tdst])
        else:
            P.op("dve", lambda e: e.memset(dst, 0.0), writes=[tdst])
            P.dma(q, lambda e: e.dma_start(out=dst[0:LR, :], in_=x[s, i * 128 - NMETA:S, :]), writes=[tdst])

    def rmsnorm_bf(src, tsrc, gb, tgb, out_bf, tout, junk, tjunk, st, tst, j, out_f32=None):
        P.op("dve", lambda e: e.scalar_tensor_tensor(out=junk, in0=src, scalar=1.0, in1=src,
                                                     op0=ALU.mult, op1=ALU.mult, accum_out=st[:, 0:1]),
             reads=[tsrc], writes=[tjunk, tst])
        rsqrt_(st[:, 1:2], st[:, 0:1], tst, 1.0 / D)
        P.op("dve", lambda e: e.tensor_scalar(out=st[:, 2:3], in0=st[:, 1:2], scalar1=valid(j), scalar2=None,
                                              op0=ALU.mult), reads=[tst, Tc], writes=[tst])
        if out_f32 is None:
            P.op("dve", lambda e: e.scalar_tensor_tensor(out=out_bf, in0=src, scalar=st[:, 2:3], in1=gb,
                                                         op0=ALU.mult, op1=ALU.mult),
                 reads=[tsrc, tst, tgb], writes=[tout])
        else:
            P.op("dve", lambda e: e.scalar_tensor_tensor(out=out_f32[0], in0=src, scalar=st[:, 2:3], in1=gb,
                                                         op0=ALU.mult, op1=ALU.mult),
                 reads=[tsrc, tst, tgb], writes=[out_f32[1]])
            P.op("pool", lambda e: e.tensor_copy(out=out_bf, in_=out_f32[0]), reads=[out_f32[1]], writes=[tout])

    def transpose_to(src_bf, tsrc, nchunk, dst, tdst, bank, kparts=128, csz=128, eng="act"):
        for c in range(nchunk):
            P.op("pe", lambda e, c=c: e.transpose(out=PSB[bank][0:csz, c * 128:(c + 1) * 128],
                                                  in_=src_bf[:, c * csz:(c + 1) * csz], identity=idb),
                 reads=[tsrc, Tc], writes=[PT_[bank]])
        srcv = mk(PSB[bank], [(128, nchunk), (1, 128)], parts=csz)
        if eng == "act":
            P.op("act", lambda e: e.copy(out=dst, in_=srcv), reads=[PT_[bank]], writes=[tdst])
        else:
            P.op("dve", lambda e: e.tensor_copy(out=dst, in_=srcv), reads=[PT_[bank]], writes=[tdst])

    try:
        for layer in range(DEPTH):
            A = Alloc(COMMON_END)
            w1 = A.get(64, F32, 33); w2 = A.get(64, F32, 64); w3 = A.get(1024, F32, 64)
            cols = A.get(4, F32, 64)
            zfT = A.get(LP, F32, 33)
            decb = A.get(1024, F32); dsk = A.get(512, F32, 1)
            hp = A.get(NT * 512, BF16); hm = A.get(NT * 512, BF16)
            a1 = A.get(512, F32, 64); h1T = A.get(512, F32, 64); h2T = A.get(512, F32, 64)
            dec = A.get(1024, F32); hf_t = A.get(512, F32); hb_t = A.get(512, F32)
            CSt = [[A.get(NT * 128, BF16) for _ in range(2)] for _ in range(2)]
            hri = [A.get(1024, F32) for _ in range(2)]
            Tw = Trk(); Ta1 = Trk(); Th1 = Trk(); Th2 = Trk(); Tdec = Trk(); Thf = Trk(); Thp = Trk()
            Tcs = [[Trk(), Trk()], [Trk(), Trk()]]; Thri = [Trk(), Trk()]
            P.dma("sp", lambda e: e.dma_start(out=w1, in_=hy_ffn_w1[layer]), writes=[Tw])
            P.dma("sp", lambda e: e.dma_start(out=w2, in_=hy_ffn_w2[layer]), writes=[Tw])
            P.dma("sp", lambda e: e.dma_start(out=w3, in_=hy_ffn_w3[layer]), writes=[Tw])
            P.dma("sp", lambda e: e.dma_start(out=cols[:, 0:1], in_=hy_ffn_b1[layer].rearrange("(p o) -> p o", o=1)), writes=[Tw])
            P.dma("sp", lambda e: e.dma_start(out=cols[:, 1:2], in_=hy_sin_freq[layer, 0].rearrange("(p o) -> p o", o=1)), writes=[Tw])
            P.dma("sp", lambda e: e.dma_start(out=cols[:, 2:3], in_=hy_ffn_b2[layer].rearrange("(p o) -> p o", o=1)), writes=[Tw])
            P.dma("sp", lambda e: e.dma_start(out=cols[:, 3:4], in_=hy_sin_freq[layer, 1].rearrange("(p o) -> p o", o=1)), writes=[Tw])
            P.dma("sp", lambda e: e.dma_start(out=zfT, in_=c_zfT), writes=[Tw])
            P.dma("sp", lambda e: e.dma_start(out=decb, in_=hy_decay[layer].rearrange("a c -> (a c)").partition_broadcast(128)), writes=[Tw])
            P.dma("sp", lambda e: e.dma_start(out=dsk, in_=hy_dskip[layer].rearrange("(o c) -> o c", o=1)), writes=[Tw])
            ki_t = A.get(512, I32, 64); kf_t = A.get(512, F32, 64)

            def sin_rr(arg, dst, tdst):
                w_ = arg.shape[1]
                P.op("dve", lambda e: e.tensor_scalar(out=arg, in0=arg, scalar1=1.0 / TWO_PI, scalar2=64.0, op0=ALU.mult, op1=ALU.add), reads=[Ta1], writes=[Ta1])
                P.op("dve", lambda e: e.tensor_copy(out=ki_t[:, 0:w_], in_=arg), reads=[Ta1], writes=[Ta1])
                P.op("dve", lambda e: e.tensor_copy(out=kf_t[:, 0:w_], in_=ki_t[:, 0:w_]), reads=[Ta1], writes=[Ta1])
                P.op("dve", lambda e: e.tensor_tensor(out=arg, in0=arg, in1=kf_t[:, 0:w_], op=ALU.subtract), reads=[Ta1], writes=[Ta1])
                P.op("dve", lambda e: e.tensor_scalar(out=arg, in0=arg, scalar1=0.499999, scalar2=-0.499999, op0=ALU.min, op1=ALU.max), reads=[Ta1], writes=[Ta1])
                P.op("act", lambda e: e.activation(out=dst, in_=arg, func=AF.Sin, scale=TWO_PI), reads=[Ta1], writes=[tdst])
            for gidx in range((NT + 3) // 4):
                j0 = gidx * 4
                nj = min(4, NT - j0)
                w = nj * 128
                cs = slice(j0 * 128, j0 * 128 + w)
                P.op("pe", lambda e: e.matmul(out=PS[0][0:64, 0:w], lhsT=w1[0:33, 0:64], rhs=zfT[0:33, cs], start=True, stop=True),
                     reads=[Tw], writes=[PT_[0]])
                P.op("dve", lambda e: e.tensor_scalar(out=a1[:, 0:w], in0=PS[0][0:64, 0:w], scalar1=cols[:, 0:1], scalar2=cols[:, 1:2],
                                                      op0=ALU.add, op1=ALU.mult), reads=[PT_[0], Tw], writes=[Ta1])
                sin_rr(a1[:, 0:w], h1T[:, 0:w], Th1)
                P.op("pe", lambda e: e.matmul(out=PS[1][0:64, 0:w], lhsT=w2[0:64, 0:64], rhs=h1T[0:64, 0:w], start=True, stop=True),
                     reads=[Tw, Th1], writes=[PT_[1]])
                P.op("dve", lambda e: e.tensor_scalar(out=a1[:, 0:w], in0=PS[1][0:64, 0:w], scalar1=cols[:, 2:3], scalar2=cols[:, 3:4],
                                                      op0=ALU.add, op1=ALU.mult), reads=[PT_[1], Tw], writes=[Ta1])
                sin_rr(a1[:, 0:w], h2T[:, 0:w], Th2)
                for jj in range(nj):
                    j = j0 + jj
                    P.op("pe", lambda e, jj=jj: e.matmul(out=PS[2][:, :], lhsT=h2T[0:64, col(jj)], rhs=w3[0:64, 0:512], start=True, stop=True),
                         reads=[Th2, Tw], writes=[PT_[2]])
                    P.op("pe", lambda e, jj=jj: e.matmul(out=PS[3][:, :], lhsT=h2T[0:64, col(jj)], rhs=w3[0:64, 512:1024], start=True, stop=True),
                         reads=[Th2, Tw], writes=[PT_[3]])
                    P.op("act", lambda e, j=j: e.activation(out=dec, in_=decb, func=AF.Exp, scale=negtn(j)), reads=[Tw, Tc], writes=[Tdec])
                    P.op("dve", lambda e, j=j: e.scalar_tensor_tensor(out=hf_t, in0=PS[2][:, :], scalar=valid(j), in1=dec[:, 0:512],
                                                                      op0=ALU.mult, op1=ALU.mult), reads=[PT_[2], Tdec, Tc], writes=[Thf])
                    P.op("dve", lambda e, j=j: e.scalar_tensor_tensor(out=hb_t, in0=PS[3][:, :], scalar=valid(j), in1=dec[:, 512:1024],
                                                                      op0=ALU.mult, op1=ALU.mult), reads=[PT_[3], Tdec, Tc], writes=[Thf])
                    if j == 0:
                        P.op("dve", lambda e: e.memset(hb_t[0:1, :], 0.0), writes=[Thf])
                        P.op("dve", lambda e: e.tensor_tensor(out=hf_t[0:1, :], in0=hf_t[0:1, :], in1=dsk[0:1, :], op=ALU.add),
                             reads=[Tw], writes=[Thf])
                    P.op("dve", lambda e, j=j: e.tensor_tensor(out=hp[:, j * 512:(j + 1) * 512], in0=hf_t, in1=hb_t, op=ALU.add),
                         reads=[Thf], writes=[Thp])
                    P.op("dve", lambda e, j=j: e.tensor_tensor(out=hm[:, j * 512:(j + 1) * 512], in0=hb_t, in1=hf_t, op=ALU.subtract),
                         reads=[Thf], writes=[Thp])
            for j in range(NT):
                b = j % 2
                P.dma("sp", lambda e, j=j, b=b: e.dma_start(out=CSt[b][0], in_=c_C[j].rearrange("p a n -> p (a n)")), writes=[Tcs[b][0]])
                P.dma("act", lambda e, j=j, b=b: e.dma_start(out=CSt[b][1], in_=c_S[j].rearrange("p a n -> p (a n)")), writes=[Tcs[b][1]])
                for a in range(NT):
                    P.op("pe", lambda e, a=a, b=b: e.matmul(out=PS[4 + b][:, :], lhsT=CSt[b][0][:, col(a)], rhs=hp[:, a * 512:(a + 1) * 512],
                                                            start=(a == 0), stop=(a == NT - 1)),
                         reads=[Tcs[b][0], Thp], writes=[PT_[4 + b]], inc=(a == NT - 1))
                for a in range(NT):
                    P.op("pe", lambda e, a=a, b=b: e.matmul(out=PS[6 + b][:, :], lhsT=CSt[b][1][:, col(a)], rhs=hm[:, a * 512:(a + 1) * 512],
                                                            start=(a == 0), stop=(a == NT - 1)),
                         reads=[Tcs[b][1], Thp], writes=[PT_[6 + b]], inc=(a == NT - 1))
                P.op("dve", lambda e, j=j, b=b: e.tensor_scalar(out=hri[b][:, 0:512], in0=PS[4 + b][:, :], scalar1=wfc(j), scalar2=None, op0=ALU.mult),
                     reads=[PT_[4 + b], Tc], writes=[Thri[b]])
                P.op("dve", lambda e, j=j, b=b: e.tensor_scalar(out=hri[b][:, 512:1024], in0=PS[6 + b][:, :], scalar1=wfc(j), scalar2=None, op0=ALU.mult),
                     reads=[PT_[6 + b], Tc], writes=[Thri[b]])
                P.dma("sp", lambda e, j=j, b=b: e.dma_start(out=HRI[j].rearrange("p a c -> p (a c)"), in_=hri[b]),
                      reads=[Thri[b]], writes=[th("HRI", j)])
            P.barrier()
            _chk("F")
            A = Alloc(COMMON_END)
            ur = [A.get(D, F32) for _ in range(2)]; vr = [A.get(D, BF16) for _ in range(2)]; utb = [A.get(D, BF16) for _ in range(2)]
            Tur = [Trk(), Trk()]; Tvr = [Trk(), Trk()]; Tutb = [Trk(), Trk()]
            for i1 in range(128):
                b = i1 % 2
                P.dma("sp", lambda e, i1=i1, b=b: e.dma_start(out=ur[b], in_=peer_u[layer, i1 * 128:(i1 + 1) * 128, :]), writes=[Tur[b]])
                P.dma("pool", lambda e, i1=i1, b=b: e.dma_start(out=vr[b], in_=peer_v[layer, i1 * 128:(i1 + 1) * 128, :]), writes=[Tvr[b]])
                P.dma("act", lambda e, i1=i1, b=b: e.dma_start(out=Vs[i1], in_=vr[b]), reads=[Tvr[b]], writes=[th("VS", i1)])
                for k in range(8):
                    bank = 2 * b + (k // 4)
                    P.op("pe", lambda e, k=k, b=b, bank=bank: e.transpose(out=PS[bank][:, (k % 4) * 128:(k % 4 + 1) * 128], in_=ur[b][:, k * 128:(k + 1) * 128], identity=idf),
                         reads=[Tur[b], Tc], writes=[PT_[bank]])
                for hb2 in range(2):
                    P.op("act", lambda e, b=b, hb2=hb2: e.copy(out=utb[b][:, hb2 * 512:(hb2 + 1) * 512], in_=PS[2 * b + hb2][:, :]), reads=[PT_[2 * b + hb2]], writes=[Tutb[b]])
                P.dma("sp", lambda e, i1=i1, b=b: e.dma_start(out=UTs[i1], in_=utb[b]), reads=[Tutb[b]], writes=[th("UT", i1)])
            P.barrier()
            _chk("U")

            for s in range(NSEQ):
                A = Alloc(COMMON_END)
                Win = A.get(8 * 2304, BF16)
                QT = A.get(8 * LP, BF16, 64); KT = A.get(2 * LP, BF16, 64); VA = A.get(NT * 130, BF16)
                g1b = A.get(D, F32); gqk = A.get(640, F32)
                gat = A.get(8, F32, 64); anorm = A.get(64, F32, 65)
                ht = [A.get(D, F32) for _ in range(2)]; junk = A.get(D, F32); st = A.get(4, F32)
                hnb = A.get(D, BF16); hnT = A.get(D, BF16)
                qk = A.get(640, F32); sq = A.get(640, F32); ssq = A.get(16, F32); qn = A.get(640, F32)
                t1 = A.get(320, F32); t2 = A.get(320, F32); qr = A.get(640, BF16)
                cs_t = [A.get(64, F32) for _ in range(2)]
                zt = [A.get(1536, F32) for _ in range(2)]
                PTb = [A.get(512, BF16) for _ in range(3)]
                oT = [A.get(512, F32, 65) for _ in range(2)]; osq = A.get(512, F32, 65); rs = A.get(512, F32, 64)
                mo = [A.get(512, BF16, 64) for _ in range(2)]
                zrow = A.get(1536, F32, 1)
                TWin = Trk(); Tg = Trk(); Tht = [Trk(), Trk()]; Tjunk = Trk(); Tst = Trk(); Thnb = Trk(); ThnT = Trk()
                Tqk = Trk(); Tsq = Trk(); Tqn = Trk(); Tt = Trk(); Tqr = Trk(); Tcs_t = [Trk(), Trk()]; Tzt = [Trk(), Trk()]
                TQ = {}; TK = {}; TV = {}
                TPT = [Trk() for _ in range(3)]; ToT = [Trk(), Trk()]; Tosq = Trk(); Trs = Trk(); Tmo = [Trk(), Trk()]
                for k in range(8):
                    for hf_ in range(2):
                        P.dma("pool", lambda e, k=k, hf_=hf_: e.dma_start(out=Win[:, k * 2304 + hf_ * 1152:k * 2304 + (hf_ + 1) * 1152],
                                                                          in_=w_in[layer, k * 128:(k + 1) * 128, hf_ * 1152:(hf_ + 1) * 1152]), writes=[TWin])
                P.dma("sp", lambda e: e.dma_start(out=g1b, in_=norm1_g[layer].partition_broadcast(128)), writes=[Tg])
                for hh in range(8):
                    P.dma("sp", lambda e, hh=hh: e.dma_start(out=gqk[:, hh * 64:(hh + 1) * 64], in_=q_norm_g[layer].partition_broadcast(128)), writes=[Tg])
                for hh in range(8, 10):
                    P.dma("sp", lambda e, hh=hh: e.dma_start(out=gqk[:, hh * 64:(hh + 1) * 64], in_=k_norm_g[layer].partition_broadcast(128)), writes=[Tg])
                P.dma("sp", lambda e: e.dma_start(out=gat, in_=attn_out_g[layer].rearrange("h d -> d h"), allow_slow_non_contiguous=True), writes=[Tg])
                P.op("dve", lambda e: e.memset(anorm[0:64, :], 1.0 / 64), writes=[Tg])
                P.op("dve", lambda e: e.memset(anorm[64:65, :], EPS), writes=[Tg])
                P.op("dve", lambda e: e.memset(VA, 1.0), writes=[Tg])
                P.op("dve", lambda e: e.memset(zrow, 0.0), writes=[Tg])
                P.dma("sp", lambda e: e.dma_start(out=Zs[s, 0:1, :], in_=zrow), reads=[Tg], writes=[th("Z", s, -1)])
                P.dma("sp", lambda e: e.dma_start(out=Zs[s, LP + 1:LP + 2, :], in_=zrow), reads=[Tg], writes=[th("Z", s, NT)])
                load_h(layer, s, 0, ht[0], Tht[0])
                for i in range(NT):
                    b = i % 2
                    if i + 1 < NT:
                        load_h(layer, s, i + 1, ht[1 - b], Tht[1 - b])
                    P.dma("act", lambda e, i=i, b=b: e.dma_start(out=cs_t[b][:, 0:32], in_=c_cos[i * 128:(i + 1) * 128, :]), writes=[Tcs_t[b]])
                    P.dma("act", lambda e, i=i, b=b: e.dma_start(out=cs_t[b][:, 32:64], in_=c_sin[i * 128:(i + 1) * 128, :]), writes=[Tcs_t[b]])
                    rmsnorm_bf(ht[b], Tht[b], g1b, Tg, hnb, Thnb, junk, Tjunk, st, Tst, i)
                    transpose_to(hnb, Thnb, 8, mk(hnT, [(128, 8), (1, 128)]), ThnT, 0)
                    for (c0, w_, bank) in ((0, 512, 1), (512, 256, 2)):
                        for k in range(8):
                            P.op("pe", lambda e, k=k, c0=c0, w_=w_, bank=bank: e.matmul(
                                out=PS[bank][:, 0:w_], lhsT=hnT[:, col(k)], rhs=Win[:, k * 2304 + c0:k * 2304 + c0 + w_],
                                start=(k == 0), stop=(k == 7)), reads=[ThnT, TWin], writes=[PT_[bank]], inc=(k == 7))
                    P.op("act", lambda e: e.copy(out=qk[:, 0:512], in_=PS[1][:, :]), reads=[PT_[1]], writes=[Tqk])
                    P.op("act", lambda e: e.copy(out=qk[:, 512:640], in_=PS[2][:, 0:128]), reads=[PT_[2]], writes=[Tqk])
                    TV[i] = Trk()
                    P.op("act", lambda e, i=i: e.copy(out=mk(VA, [(65, 2), (1, 64)], off=i * 130), in_=mk(PS[2], [(64, 2), (1, 64)], off=128)),
                         reads=[PT_[2], Tg], writes=[TV[i]])
                    for zi in range(3):
                        bank = 3 + zi
                        for k in range(8):
                            P.op("pe", lambda e, k=k, zi=zi, bank=bank: e.matmul(
                                out=PS[bank][:, :], lhsT=hnT[:, col(k)], rhs=Win[:, k * 2304 + 768 + zi * 512:k * 2304 + 768 + (zi + 1) * 512],
                                start=(k == 0), stop=(k == 7)), reads=[ThnT, TWin], writes=[PT_[bank]], inc=(k == 7))
                        P.op("act", lambda e, zi=zi, bank=bank, b=b: e.copy(out=zt[b][:, zi * 512:(zi + 1) * 512], in_=PS[bank][:, :]),
                             reads=[PT_[bank]], writes=[Tzt[b]])
                    P.dma("sp", lambda e, i=i, b=b: e.dma_start(out=Zs[s, 1 + i * 128:1 + (i + 1) * 128, :], in_=zt[b]),
                          reads=[Tzt[b]], writes=[th("Z", s, i)])
                    P.op("dve", lambda e: e.tensor_tensor(out=sq, in0=qk, in1=qk, op=ALU.mult), reads=[Tqk], writes=[Tsq])
                    P.op("dve", lambda e: e.tensor_reduce(out=ssq[:, 0:10], in_=mk(sq, [(64, 10), (1, 64)]), axis=AX.X, op=ALU.add),
                         reads=[Tsq], writes=[Tst])
                    rsqrt_(ssq[:, 0:10], ssq[:, 0:10], Tst, 1.0 / 64)
                    P.op("dve", lambda e: e.tensor_tensor(out=mk(qn, [(64, 10), (1, 64)]), in0=mk(qk, [(64, 10), (1, 64)]),
                                                          in1=mk(ssq, [(1, 10), (0, 64)]), op=ALU.mult), reads=[Tqk, Tst], writes=[Tqn])
                    P.op("dve", lambda e: e.tensor_tensor(out=qn, in0=qn, in1=gqk, op=ALU.mult), reads=[Tg], writes=[Tqn])
                    x0 = mk(qn, [(64, 10), (2, 32)]); x1 = mk(qn, [(64, 10), (2, 32)], off=1)
                    cb = mk(cs_t[b], [(0, 10), (1, 32)]); sb_ = mk(cs_t[b], [(0, 10), (1, 32)], off=32)
                    o0 = mk(qr, [(64, 10), (2, 32)]); o1 = mk(qr, [(64, 10), (2, 32)], off=1)
                    t1v = mk(t1, [(32, 10), (1, 32)]); t2v = mk(t2, [(32, 10), (1, 32)])
                    P.op("dve", lambda e, x0=x0, cb=cb: e.tensor_tensor(out=t1v, in0=x0, in1=cb, op=ALU.mult), reads=[Tqn, Tcs_t[b]], writes=[Tt])
                    P.op("dve", lambda e, x1=x1, sb_=sb_: e.tensor_tensor(out=t2v, in0=x1, in1=sb_, op=ALU.mult), reads=[Tqn, Tcs_t[b]], writes=[Tt])
                    P.op("dve", lambda e, o0=o0: e.tensor_tensor(out=o0, in0=t1v, in1=t2v, op=ALU.subtract), reads=[Tt], writes=[Tqr])
                    P.op("dve", lambda e, x0=x0, sb_=sb_: e.tensor_tensor(out=t1v, in0=x0, in1=sb_, op=ALU.mult), reads=[Tqn, Tcs_t[b]], writes=[Tt])
                    P.op("dve", lambda e, x1=x1, cb=cb: e.tensor_tensor(out=t2v, in0=x1, in1=cb, op=ALU.mult), reads=[Tqn, Tcs_t[b]], writes=[Tt])
                    P.op("dve", lambda e, o1=o1: e.tensor_tensor(out=o1, in0=t1v, in1=t2v, op=ALU.add), reads=[Tt], writes=[Tqr])
                    TQ[i] = Trk(); TK[i] = Trk()
                    transpose_to(qr, Tqr, 8, mk(QT, [(LP, 8), (1, 128)], off=i * 128), TQ[i], 6, csz=64)
                    transpose_to(qr[:, 512:640], Tqr, 2, mk(KT, [(LP, 2), (1, 128)], off=i * 128), TK[i], 7, csz=64, eng="dve")
                _chk("A")
                it = 0
                for qb in range(NT):
                    for g in range(2):
                        ob = 6 + g
                        for sbk in range(NT):
                            ks = 128 if sbk < NT - 1 else LR
                            sb_bank = it % 3
                            pb = it % 3
                            it += 1
                            P.op("pe", lambda e, g=g, sbk=sbk, ks=ks, qb=qb, sb_bank=sb_bank: e.matmul(
                                out=PS[sb_bank][0:ks, :], lhsT=KT[0:64, g * LP + sbk * 128:g * LP + sbk * 128 + ks],
                                rhs=mk(QT, [(LP, 4), (1, 128)], off=4 * g * LP + qb * 128), start=True, stop=True),
                                reads=[TK[sbk], TQ[qb]], writes=[PT_[sb_bank]])
                            P.op("act", lambda e, ks=ks, sb_bank=sb_bank, pb=pb: e.activation(
                                out=PTb[pb][0:ks, :], in_=PS[sb_bank][0:ks, :], func=AF.Exp, scale=0.125),
                                reads=[PT_[sb_bank]], writes=[TPT[pb]])
                            P.op("pe", lambda e, g=g, sbk=sbk, ks=ks, pb=pb, ob=ob: e.matmul(
                                out=PS[ob][0:65, :], lhsT=VA[0:ks, sbk * 130 + g * 65:sbk * 130 + g * 65 + 65], rhs=PTb[pb][0:ks, :],
                                start=(sbk == 0), stop=(sbk == NT - 1)), reads=[TV[sbk], TPT[pb]], writes=[PT_[ob]], inc=True)
                        P.op("dve", lambda e, g=g, ob=ob: e.tensor_copy(out=oT[g], in_=PS[ob][0:65, :]), reads=[PT_[ob]], writes=[ToT[g]])
                        P.op("dve", lambda e, g=g: e.tensor_tensor(out=osq, in0=oT[g], in1=oT[g], op=ALU.mult), reads=[ToT[g]], writes=[Tosq])
                        P.op("pe", lambda e: e.matmul(out=PS[3][0:64, :], lhsT=anorm[0:65, 0:64], rhs=osq[0:65, :], start=True, stop=True),
                             reads=[Tg, Tosq], writes=[PT_[3]])
                        rsqrt_(rs, PS[3][0:64, :], Trs, 1.0, use_eps=False, extra_reads=[PT_[3]])
                        for jh in range(4):
                            hh = 4 * g + jh
                            P.op("dve", lambda e, g=g, jh=jh, hh=hh: e.scalar_tensor_tensor(
                                out=mo[g][:, col(jh)], in0=oT[g][0:64, col(jh)], scalar=gat[:, hh:hh + 1], in1=rs[:, col(jh)],
                                op0=ALU.mult, op1=ALU.mult), reads=[ToT[g], Trs, Tg], writes=[Tmo[g]])
                        P.dma("sp", lambda e, g=g, qb=qb: e.dma_start(out=MIXA[s, :, 4 * g:4 * g + 4, qb * 128:(qb + 1) * 128],
                                                                     in_=mk(mo[g], [(128, 4), (1, 128)])),
                              reads=[Tmo[g]], writes=[th("MIXA", s, qb)])
                P.barrier()
                _chk("B")

                A = Alloc(COMMON_END)
                u_sb = A.get(NT * 512, BF16)
                C2_START = A.off
                zm = A.get(1536, F32); z0 = A.get(1536, F32); zp = A.get(1536, F32); acc = A.get(1536, F32)
                wt = [A.get(1536, F32) for _ in range(4)]
                x1t = A.get(512, F32); uf = A.get(512, F32)
                Twt = Trk(); Tz3 = Trk(); Tacc = Trk(); Tx1 = Trk(); Tuf = Trk(); Tu = Trk()
                for r in range(3):
                    P.dma("sp", lambda e, r=r: e.dma_start(out=wt[r], in_=hy_conv_w[layer, r].partition_broadcast(128)), writes=[Twt])
                P.dma("sp", lambda e: e.dma_start(out=wt[3], in_=hy_conv_b[layer].partition_broadcast(128)), writes=[Twt])
                for i in range(NT):
                    r0 = 1 + i * 128
                    zdeps = [th("Z", s, i - 1), th("Z", s, i), th("Z", s, min(i + 1, NT))]
                    P.dma("sp", lambda e, r0=r0: e.dma_start(out=zm, in_=Zs[s, r0 - 1:r0 + 127, :]), reads=zdeps, writes=[Tz3])
                    P.dma("act", lambda e, r0=r0: e.dma_start(out=z0, in_=Zs[s, r0:r0 + 128, :]), reads=zdeps, writes=[Tz3])
                    P.dma("sp", lambda e, r0=r0: e.dma_start(out=zp, in_=Zs[s, r0 + 1:r0 + 129, :]), reads=zdeps, writes=[Tz3])
                    P.op("dve", lambda e: e.tensor_tensor(out=acc, in0=zm, in1=wt[0], op=ALU.mult), reads=[Tz3, Twt], writes=[Tacc])
                    P.op("dve", lambda e: e.tensor_tensor(out=z0, in0=z0, in1=wt[1], op=ALU.mult), reads=[Twt], writes=[Tz3])
                    P.op("dve", lambda e: e.tensor_tensor(out=zp, in0=zp, in1=wt[2], op=ALU.mult), reads=[Twt], writes=[Tz3])
                    P.op("dve", lambda e: e.tensor_tensor(out=acc, in0=acc, in1=z0, op=ALU.add), reads=[Tz3], writes=[Tacc])
                    P.op("dve", lambda e: e.tensor_tensor(out=acc, in0=acc, in1=zp, op=ALU.add), reads=[Tz3], writes=[Tacc])
                    P.op("dve", lambda e: e.tensor_tensor(out=acc, in0=acc, in1=wt[3], op=ALU.add), reads=[Twt], writes=[Tacc])
                    P.op("act", lambda e: e.copy(out=x1t, in_=acc[:, 512:1024]), reads=[Tacc], writes=[Tx1])
                    P.dma("sp", lambda e, i=i: e.dma_start(out=X1s[s, i * 128:(i + 1) * 128, :], in_=x1t), reads=[Tx1], writes=[th("X1", s, i)])
                    P.op("dve", lambda e, i=i: e.scalar_tensor_tensor(out=u_sb[:, i * 512:(i + 1) * 512], in0=acc[:, 1024:1536], scalar=valid(i),
                                                                      in1=acc[:, 0:512], op0=ALU.mult, op1=ALU.mult),
                         reads=[Tacc, Tc], writes=[Tu])
                P.barrier()
                _chk("C1")
                A = Alloc(C2_START)
                Y = A.get(NT * 1024, BF16)
                CSt = [[A.get(NT * 128, BF16) for _ in range(2)] for _ in range(2)]
                hri = [A.get(1024, F32) for _ in range(2)]
                ab = [A.get(1024, F32) for _ in range(2)]
                m1 = A.get(512, F32); m2 = A.get(512, F32)
                gyb = A.get(512, F32); x1l = [A.get(512, F32) for _ in range(2)]
                oy = A.get(512, F32); osq2 = A.get(512, F32); st8 = A.get(8, F32); ob16 = A.get(512, BF16)
                mh = [A.get(512, BF16) for _ in range(2)]
                Tcs = [[Trk(), Trk()], [Trk(), Trk()]]; Thri = [Trk(), Trk()]; Tab = [Trk(), Trk()]; Tm = Trk()
                TY = Trk(); Tgy = Trk(); Tx1l = [Trk(), Trk()]; Toy = Trk(); Tst8 = Trk(); Tob = Trk(); Tmh = [Trk(), Trk()]
                P.dma("sp", lambda e: e.dma_start(out=gyb, in_=hy_out_g[layer].rearrange("g d -> (g d)").partition_broadcast(128)), writes=[Tgy])
                for j in range(NT):
                    b = j % 2
                    P.dma("sp", lambda e, j=j, b=b: e.dma_start(out=CSt[b][0], in_=c_C[j].rearrange("p a n -> p (a n)")), writes=[Tcs[b][0]])
                    P.dma("act", lambda e, j=j, b=b: e.dma_start(out=CSt[b][1], in_=c_S[j].rearrange("p a n -> p (a n)")), writes=[Tcs[b][1]])
                    P.dma("sp", lambda e, j=j, b=b: e.dma_start(out=hri[b], in_=HRI[j].rearrange("p a c -> p (a c)")), reads=[th("HRI", j)], writes=[Thri[b]])
                    for cs_i in range(2):
                        bank = 2 * b + cs_i
                        for a in range(NT):
                            P.op("pe", lambda e, a=a, b=b, cs_i=cs_i, bank=bank: e.matmul(
                                out=PS[bank][:, :], lhsT=CSt[b][cs_i][:, col(a)], rhs=u_sb[:, a * 512:(a + 1) * 512],
                                start=(a == 0), stop=(a == NT - 1)), reads=[Tcs[b][cs_i], Tu], writes=[PT_[bank]], inc=(a == NT - 1))
                        P.op("act", lambda e, b=b, cs_i=cs_i, bank=bank: e.copy(out=ab[b][:, cs_i * 512:(cs_i + 1) * 512], in_=PS[bank][:, :]),
                             reads=[PT_[bank]], writes=[Tab[b]])
                    Av = ab[b][:, 0:512]; Bv = ab[b][:, 512:1024]; Hr = hri[b][:, 0:512]; Hi = hri[b][:, 512:1024]
                    P.op("dve", lambda e, Av=Av, Hr=Hr: e.tensor_tensor(out=m1, in0=Av, in1=Hr, op=ALU.mult), reads=[Tab[b], Thri[b]], writes=[Tm])
                    P.op("pool", lambda e, Bv=Bv, Hi=Hi: e.tensor_tensor(out=m2, in0=Bv, in1=Hi, op=ALU.mult), reads=[Tab[b], Thri[b]], writes=[Tm])
                    P.op("dve", lambda e, j=j: e.tensor_tensor(out=Y[:, j * 1024:j * 1024 + 512], in0=m1, in1=m2, op=ALU.add), reads=[Tm], writes=[TY])
                    P.op("dve", lambda e, Bv=Bv, Hr=Hr: e.tensor_tensor(out=m1, in0=Bv, in1=Hr, op=ALU.mult), reads=[Tab[b], Thri[b]], writes=[Tm])
                    P.op("pool", lambda e, Av=Av, Hi=Hi: e.tensor_tensor(out=m2, in0=Av, in1=Hi, op=ALU.mult), reads=[Tab[b], Thri[b]], writes=[Tm])
                    P.op("dve", lambda e, j=j: e.tensor_tensor(out=Y[:, j * 1024 + 512:(j + 1) * 1024], in0=m1, in1=m2, op=ALU.subtract), reads=[Tm], writes=[TY])
                for j in range(NT):
                    b = j % 2
                    P.dma("sp", lambda e, j=j, b=b: e.dma_start(out=CSt[b][0], in_=c_C[j].rearrange("p a n -> p (a n)")), writes=[Tcs[b][0]])
                    P.dma("act", lambda e, j=j, b=b: e.dma_start(out=CSt[b][1], in_=c_S[j].rearrange("p a n -> p (a n)")), writes=[Tcs[b][1]])
                    P.dma("sp", lambda e, j=j, b=b: e.dma_start(out=x1l[b], in_=X1s[s, j * 128:(j + 1) * 128, :]), reads=[th("X1", s, j)], writes=[Tx1l[b]])
                    bank = 4 + b
                    for a in range(NT):
                        P.op("pe", lambda e, a=a, b=b, bank=bank: e.matmul(out=PS[bank][:, :], lhsT=CSt[b][0][:, col(a)], rhs=Y[:, a * 1024:a * 1024 + 512],
                                                                           start=(a == 0), stop=False), reads=[Tcs[b][0], TY], writes=[PT_[bank]], inc=False)
                    for a in range(NT):
                        P.op("pe", lambda e, a=a, b=b, bank=bank: e.matmul(out=PS[bank][:, :], lhsT=CSt[b][1][:, col(a)], rhs=Y[:, a * 1024 + 512:(a + 1) * 1024],
                                                                           start=False, stop=(a == NT - 1)), reads=[Tcs[b][1], TY], writes=[PT_[bank]], inc=(a == NT - 1))
                    P.op("dve", lambda e, b=b, bank=bank: e.tensor_tensor(out=oy, in0=PS[bank][:, :], in1=x1l[b], op=ALU.mult), reads=[PT_[bank], Tx1l[b]], writes=[Toy])
                    P.op("pool", lambda e: e.tensor_tensor(out=osq2, in0=oy, in1=oy, op=ALU.mult), reads=[Toy], writes=[Tst8])
                    P.op("dve", lambda e: e.tensor_reduce(out=st8, in_=mk(osq2, [(64, 8), (1, 64)]), axis=AX.X, op=ALU.add), reads=[Tst8], writes=[Tst8])
                    rsqrt_(st8, st8, Tst8, 1.0 / 64)
                    P.op("dve", lambda e: e.tensor_tensor(out=mk(oy, [(64, 8), (1, 64)]), in0=mk(oy, [(64, 8), (1, 64)]), in1=mk(st8, [(1, 8), (0, 64)]), op=ALU.mult),
                         reads=[Tst8], writes=[Toy])
                    P.op("dve", lambda e: e.tensor_tensor(out=ob16, in0=oy, in1=gyb, op=ALU.mult), reads=[Toy, Tgy], writes=[Tob])
                    transpose_to(ob16, Tob, 4, mk(mh[b], [(128, 4), (1, 128)]), Tmh[b], 6 + b, eng="act")
                    P.dma("sp", lambda e, j=j, b=b: e.dma_start(out=MIXH[s, :, :, j * 128:(j + 1) * 128], in_=mk(mh[b], [(128, 4), (1, 128)])),
                          reads=[Tmh[b]], writes=[th("MIXH", s, j)])
                P.barrier()
                _chk("C2")

                A = Alloc(COMMON_END)
                Woa = A.get(8 * 1024, BF16, 64); Woh = A.get(4 * 1024, BF16)
                wq = A.get(8 * 2048, BF16); keysT = A.get(16 * 128, BF16); kst = A.get(16 * 128, F32)
                g2b = A.get(D, F32); gfb = A.get(D, F32)
                ht = [A.get(D, F32) for _ in range(2)]; h2 = A.get(D, F32); junk = A.get(D, F32); st = A.get(4, F32)
                hn2b = A.get(D, BF16); hn2f = A.get(D, F32); hn2T = A.get(D, BF16)
                mat = [A.get(8 * 128, BF16, 64) for _ in range(2)]; mht = [A.get(4 * 128, BF16) for _ in range(2)]
                qT = A.get(16 * 128, BF16)
                s_sb = A.get(2048, F32); s2_sb = A.get(2048, F32)
                sv = A.get(256, F32); si = A.get(256, U32); sif = A.get(256, F32)
                cand = A.get(2048, F32); cand2 = A.get(2048, F32)
                tv = A.get(128, F32); tj = A.get(128, U32); tjf = A.get(128, F32); k1f = A.get(128, F32); k2f = A.get(128, F32); k1i = A.get(128, I32)
                eq = A.get(2048, F32); i1s = A.get(128, F32); i2s = A.get(128, F32); eidf = A.get(128, F32); eid = A.get(128, U32)
                nm = A.get(8, F32); Zs_ = A.get(8, F32); gE = A.get(128, F32); av = A.get(128, F32); wv = A.get(128, F32)
                rtT = A.get(384, F32); TrtT = Trk()
                TW = Trk(); Tht = [Trk(), Trk()]; Th2 = Trk(); Tjunk = Trk(); Tst = Trk(); Thn = Trk(); ThnT = Trk(); Thnf = Trk()
                Tmat = [Trk(), Trk()]; Tmht = [Trk(), Trk()]; TqT = Trk(); Ts = Trk(); Ts2 = Trk(); Tsv = Trk(); Tcand = Trk()
                Ttv = Trk(); Tk = Trk(); Teq = Trk(); Teid = Trk(); Tgw = Trk(); Tav = Trk()
                Tpacc = Trk(); Tyo = Trk(); Tkst = Trk()
                for hh in range(8):
                    P.dma("pool", lambda e, hh=hh: e.dma_start(out=Woa[:, hh * 1024:(hh + 1) * 1024], in_=w_out[layer, hh * 64:(hh + 1) * 64, :]), writes=[TW])
                for c in range(4):
                    P.dma("pool", lambda e, c=c: e.dma_start(out=Woh[:, c * 1024:(c + 1) * 1024], in_=w_out[layer, 512 + c * 128:512 + (c + 1) * 128, :]), writes=[TW])
                for k in range(8):
                    P.dma("pool", lambda e, k=k: e.dma_start(out=wq[:, k * 2048:(k + 1) * 2048], in_=peer_wq[layer, k * 128:(k + 1) * 128, :]), writes=[TW])
                P.dma("sp", lambda e: e.dma_start(out=g2b, in_=norm2_g[layer].partition_broadcast(128)), writes=[TW])
                P.dma("sp", lambda e: e.dma_start(out=gfb, in_=final_g.partition_broadcast(128)), writes=[TW])
                for hh in range(8):
                    for pp in range(2):
                        ci = 2 * hh + pp
                        P.dma("sp", lambda e, hh=hh, pp=pp, ci=ci: e.dma_start(out=kst[:, ci * 128:(ci + 1) * 128], in_=peer_keys[layer, pp, hh]), writes=[Tkst])
                for ci in range(16):
                    bank = ci % 2
                    P.op("pe", lambda e, ci=ci, bank=bank: e.transpose(out=PS[bank][:, 0:128], in_=kst[:, ci * 128:(ci + 1) * 128], identity=idf),
                         reads=[Tkst, Tc], writes=[PT_[bank]])
                    P.op("act", lambda e, ci=ci, bank=bank: e.copy(out=keysT[:, ci * 128:(ci + 1) * 128], in_=PS[bank][:, 0:128]), reads=[PT_[bank]], writes=[TW])
                load_h(layer, s, 0, ht[0], Tht[0])
                for i in range(NT):
                    b = i % 2
                    if i + 1 < NT:
                        load_h(layer, s, i + 1, ht[1 - b], Tht[1 - b])
                    P.dma("act", lambda e, i=i, b=b: e.dma_start(out=mk(mat[b], [(128, 8), (1, 128)]), in_=MIXA[s, :, :, i * 128:(i + 1) * 128]),
                          reads=[th("MIXA", s, i)], writes=[Tmat[b]])
                    P.dma("act", lambda e, i=i, b=b: e.dma_start(out=mk(mht[b], [(128, 4), (1, 128)]), in_=MIXH[s, :, :, i * 128:(i + 1) * 128]),
                          reads=[th("MIXH", s, i)], writes=[Tmht[b]])
                    for cg in range(2):
                        bank = cg
                        for hh in range(8):
                            P.op("pe", lambda e, hh=hh, cg=cg, b=b, bank=bank: e.matmul(
                                out=PS[bank][:, :], lhsT=mat[b][0:64, col(hh)], rhs=Woa[0:64, hh * 1024 + cg * 512:hh * 1024 + (cg + 1) * 512],
                                start=(hh == 0), stop=False), reads=[Tmat[b], TW], writes=[PT_[bank]], inc=False)
                        for c in range(4):
                            P.op("pe", lambda e, c=c, cg=cg, b=b, bank=bank: e.matmul(
                                out=PS[bank][:, :], lhsT=mht[b][:, col(c)], rhs=Woh[:, c * 1024 + cg * 512:c * 1024 + (cg + 1) * 512],
                                start=False, stop=(c == 3)), reads=[Tmht[b], TW], writes=[PT_[bank]], inc=(c == 3))
                        P.op("dve", lambda e, cg=cg, b=b, bank=bank: e.tensor_tensor(out=h2[:, cg * 512:(cg + 1) * 512], in0=PS[bank][:, :],
                                                                                     in1=ht[b][:, cg * 512:(cg + 1) * 512], op=ALU.add),
                             reads=[PT_[bank], Tht[b]], writes=[Th2])
                    rmsnorm_bf(h2, Th2, g2b, TW, hn2b, Thn, junk, Tjunk, st, Tst, i, out_f32=(hn2f, Thnf))
                    transpose_to(hn2b, Thn, 8, mk(hn2T, [(128, 8), (1, 128)]), ThnT, 2)
                    for c4 in range(4):
                        bank = 3 + (c4 % 2)
                        for cc in range(4):
                            ci = c4 * 4 + cc
                            for k in range(8):
                                P.op("pe", lambda e, ci=ci, cc=cc, k=k, bank=bank: e.matmul(
                                    out=PS[bank][:, col(cc)], lhsT=wq[:, k * 2048 + ci * 128:k * 2048 + (ci + 1) * 128], rhs=hn2T[:, col(k)],
                                    start=(k == 0), stop=(k == 7)), reads=[TW, ThnT], writes=[PT_[bank]], inc=(k == 7 and cc == 3))
                        P.op("act", lambda e, c4=c4, bank=bank: e.copy(out=qT[:, c4 * 512:(c4 + 1) * 512], in_=PS[bank][:, :]), reads=[PT_[bank]], writes=[TqT])
                    for c4 in range(4):
                        bank = 5 + (c4 % 2)
                        for cc in range(4):
                            ci = c4 * 4 + cc
                            P.op("pe", lambda e, ci=ci, cc=cc, bank=bank: e.matmul(out=PS[bank][:, col(cc)], lhsT=qT[:, col(ci)], rhs=keysT[:, col(ci)],
                                                                                    start=True, stop=True), reads=[TqT, TW], writes=[PT_[bank]], inc=(cc == 3))
                        P.op("act", lambda e, c4=c4, bank=bank: e.copy(out=s_sb[:, c4 * 512:(c4 + 1) * 512], in_=PS[bank][:, :]), reads=[PT_[bank]], writes=[Ts])
                    for ci in range(16):
                        sl = s_sb[:, col(ci)]; sl2 = s2_sb[:, col(ci)]
                        P.op("dve", lambda e, ci=ci, sl=sl: e.max(out=sv[:, ci * 16:ci * 16 + 8], in_=sl), reads=[Ts], writes=[Tsv])
                        P.op("dve", lambda e, ci=ci, sl=sl: e.max_index(out=si[:, ci * 16:ci * 16 + 8], in_max=sv[:, ci * 16:ci * 16 + 8], in_values=sl), reads=[Ts], writes=[Tsv])
                        P.op("dve", lambda e, ci=ci, sl=sl, sl2=sl2: e.match_replace(out=sl2, in_to_replace=sv[:, ci * 16:ci * 16 + 8], in_values=sl, imm_value=-1e30),
                             reads=[Ts, Tsv], writes=[Ts2])
                        P.op("dve", lambda e, ci=ci, sl2=sl2: e.max(out=sv[:, ci * 16 + 8:ci * 16 + 16], in_=sl2), reads=[Ts2], writes=[Tsv])
                        P.op("dve", lambda e, ci=ci, sl2=sl2: e.max_index(out=si[:, ci * 16 + 8:ci * 16 + 16], in_max=sv[:, ci * 16 + 8:ci * 16 + 16], in_values=sl2),
                             reads=[Ts2], writes=[Tsv])
                    P.op("dve", lambda e: e.tensor_copy(out=sif, in_=si), reads=[Tsv], writes=[Tsv])
                    s1b = mk(sv, [(32, 8), (1, 16), (0, 16)]); s2b = mk(sv, [(32, 8), (0, 16), (1, 16)], off=16)
                    P.op("dve", lambda e, s1b=s1b, s2b=s2b: e.tensor_tensor(out=mk(cand, [(256, 8), (16, 16), (1, 16)]), in0=s1b, in1=s2b, op=ALU.add),
                         reads=[Tsv], writes=[Tcand])
                    for hh in range(8):
                        cl = cand[:, hh * 256:(hh + 1) * 256]; cl2 = cand2[:, hh * 256:(hh + 1) * 256]
                        P.op("dve", lambda e, hh=hh, cl=cl: e.max(out=tv[:, hh * 16:hh * 16 + 8], in_=cl), reads=[Tcand], writes=[Ttv])
                        P.op("dve", lambda e, hh=hh, cl=cl: e.max_index(out=tj[:, hh * 16:hh * 16 + 8], in_max=tv[:, hh * 16:hh * 16 + 8], in_values=cl), reads=[Tcand], writes=[Ttv])
                        P.op("dve", lambda e, hh=hh, cl=cl, cl2=cl2: e.match_replace(out=cl2, in_to_replace=tv[:, hh * 16:hh * 16 + 8], in_values=cl, imm_value=-1e30),
                             reads=[Tcand, Ttv], writes=[Ts2])
                        P.op("dve", lambda e, hh=hh, cl2=cl2: e.max(out=tv[:, hh * 16 + 8:hh * 16 + 16], in_=cl2), reads=[Ts2], writes=[Ttv])
                        P.op("dve", lambda e, hh=hh, cl2=cl2: e.max_index(out=tj[:, hh * 16 + 8:hh * 16 + 16], in_max=tv[:, hh * 16 + 8:hh * 16 + 16], in_values=cl2),
                             reads=[Ts2], writes=[Ttv])
                    P.op("dve", lambda e: e.tensor_copy(out=tjf, in_=tj), reads=[Ttv], writes=[Tk])
                    P.op("dve", lambda e: e.tensor_scalar(out=k1f, in0=tjf, scalar1=1.0 / 16, scalar2=-0.46875, op0=ALU.mult, op1=ALU.add), reads=[Tk], writes=[Tk])
                    P.op("dve", lambda e: e.tensor_copy(out=k1i, in_=k1f), reads=[Tk], writes=[Tk])
                    P.op("dve", lambda e: e.tensor_copy(out=k1f, in_=k1i), reads=[Tk], writes=[Tk])
                    P.op("dve", lambda e: e.scalar_tensor_tensor(out=k2f, in0=k1f, scalar=-16.0, in1=tjf, op0=ALU.mult, op1=ALU.add), reads=[Tk], writes=[Tk])
                    iob = mk(iota16, [(0, 128), (1, 16)])
                    for (kf, ioff, dst) in ((k1f, 0, i1s), (k2f, 16, i2s)):
                        P.op("dve", lambda e, kf=kf: e.tensor_tensor(out=mk(eq, [(16, 128), (1, 16)]), in0=iob, in1=mk(kf, [(1, 128), (0, 16)]), op=ALU.is_equal),
                             reads=[Tk, Tc], writes=[Teq])
                        P.op("dve", lambda e, ioff=ioff: e.tensor_tensor(out=mk(eq, [(256, 8), (16, 16), (1, 16)]), in0=mk(eq, [(256, 8), (16, 16), (1, 16)]),
                                                                         in1=mk(sif, [(32, 8), (0, 16), (1, 16)], off=ioff), op=ALU.mult), reads=[Tsv], writes=[Teq])
                        P.op("dve", lambda e, dst=dst: e.tensor_reduce(out=dst, in_=mk(eq, [(16, 128), (1, 16)]), axis=AX.X, op=ALU.add), reads=[Teq], writes=[Teid])
                    P.op("dve", lambda e: e.tensor_scalar(out=nm, in0=mk(tv, [(16, 8)]), scalar1=-1.0, scalar2=None, op0=ALU.mult), reads=[Ttv], writes=[Tgw])
                    for hh in range(8):
                        P.op("act", lambda e, hh=hh: e.activation(out=gE[:, hh * 16:(hh + 1) * 16], in_=tv[:, hh * 16:(hh + 1) * 16], func=AF.Exp,
                                                                  bias=nm[:, hh:hh + 1], scale=1.0), reads=[Ttv, Tgw], writes=[Tgw])
                    P.op("dve", lambda e: e.tensor_reduce(out=Zs_, in_=mk(gE, [(16, 8), (1, 16)]), axis=AX.X, op=ALU.add), reads=[Tgw], writes=[Tgw])
                    P.op("dve", lambda e: e.reciprocal(out=Zs_, in_=Zs_), reads=[Tgw], writes=[Tgw])
                    P.op("dve", lambda e: e.tensor_tensor(out=mk(gE, [(16, 8), (1, 16)]), in0=mk(gE, [(16, 8), (1, 16)]), in1=mk(Zs_, [(1, 8), (0, 16)]), op=ALU.mult),
                         reads=[Tgw], writes=[Tgw])
                    P.dma("sp", lambda e, i=i: e.dma_start(out=H2s[s, i * 128:(i + 1) * 128, :], in_=h2), reads=[Th2], writes=[th("H2", s, i)])
                    P.dma("act", lambda e, i=i: e.dma_start(out=HN2T[s, :, :, i * 128:(i + 1) * 128], in_=mk(hn2T, [(128, 8), (1, 128)])),
                          reads=[ThnT], writes=[th("HN2T", s, i)])
                    for c3, (src_, tsrc_) in enumerate(((i1s, Teid), (i2s, Teid), (gE, Tgw))):
                        P.op("pe", lambda e, c3=c3, src_=src_: e.transpose(out=PS[7][:, c3 * 128:(c3 + 1) * 128], in_=src_, identity=idf),
                             reads=[tsrc_, Tc], writes=[PT_[7]])
                    P.op("act", lambda e: e.copy(out=rtT, in_=PS[7][:, 0:384]), reads=[PT_[7]], writes=[TrtT])
                    P.dma("sp", lambda e, i=i: e.dma_start(out=RT[s, i], in_=rtT), reads=[TrtT], writes=[th("RT", s, i)])
                P.barrier()
                _chk("D1")
                A = Alloc(COMMON_END)
                GT = 512
                NBUF = 5
                OB = 16
                AT = A.get(128 * GT, BF16); hng = A.get(8 * GT, BF16)
                UTc = [A.get(1024, BF16) for _ in range(NBUF)]; Vc = [A.get(1024, BF16) for _ in range(NBUF)]
                rtl = [A.get(384, F32) for _ in range(2)]; rtb = A.get(256, BF16)
                OH1 = [A.get(OB * 128, BF16) for _ in range(2)]; OH2 = [A.get(OB * 128, BF16) for _ in range(2)]; iob128 = A.get(128, BF16)
                gfb = A.get(D, F32); h2l = [A.get(D, F32) for _ in range(2)]; po = A.get(D, F32); st = A.get(4, F32); yo = A.get(D, F32); junk = yo
                TAT = Trk(); Thng = Trk(); TUT = [Trk() for _ in range(NBUF)]; TVc = [Trk() for _ in range(NBUF)]; Trtl = [Trk(), Trk()]; Trtb = Trk()
                TOH1 = [Trk(), Trk()]; TOH2 = [Trk(), Trk()]; Tio = Trk(); TW = Trk(); Th2l = [Trk(), Trk()]; Tpo = Trk(); Tjunk = Trk(); Tst = Trk(); Tyo = Trk()
                P.dma("sp", lambda e: e.dma_start(out=gfb, in_=final_g.partition_broadcast(128)), writes=[TW])
                P.dma("sp", lambda e: e.dma_start(out=iob128, in_=c_io128), writes=[Tio])
                Tjunk = Tyo
                iobv = mk(iob128, [(0, OB), (1, 128)])
                groups = [list(range(t0_, min(t0_ + 4, NT))) for t0_ in range(0, NT, 4)]
                obn = 0
                dq = 0
                for tiles in groups:
                    nt_ = len(tiles); W_ = nt_ * 128; tok0 = tiles[0] * 128
                    P.dma("sp", lambda e, W_=W_, tok0=tok0: e.dma_start(out=mk(hng, [(GT, 8), (1, W_)]), in_=HN2T[s, :, :, tok0:tok0 + W_]),
                          reads=[th("HN2T", s, t_) for t_ in tiles], writes=[Thng])
                    for i1 in range(128):
                        ub = i1 % NBUF; bk = i1 % 2
                        dq += 1
                        P.dma("sp" if dq % 2 else "act", lambda e, i1=i1, ub=ub: e.dma_start(out=UTc[ub], in_=UTs[i1]), reads=[th("UT", i1)], writes=[TUT[ub]])
                        for k in range(8):
                            P.op("pe", lambda e, k=k, ub=ub, bk=bk, W_=W_: e.matmul(out=PS[bk][:, 0:W_], lhsT=UTc[ub][:, k * 128:(k + 1) * 128],
                                                                                    rhs=hng[:, k * GT:k * GT + W_], start=(k == 0), stop=(k == 7)),
                                 reads=[TUT[ub], Thng], writes=[PT_[bk]], inc=(k == 7))
                        P.op("act", lambda e, i1=i1, bk=bk, W_=W_: e.activation(out=AT[:, i1 * GT:i1 * GT + W_], in_=PS[bk][:, 0:W_], func=AF.Gelu),
                             reads=[PT_[bk]], writes=[TAT])
                    for tt, tile in enumerate(tiles):
                        rb = tile % 2
                        P.dma("sp", lambda e, tile=tile, rb=rb: e.dma_start(out=rtl[rb], in_=RT[s, tile]), reads=[th("RT", s, tile)], writes=[Trtl[rb]])
                        P.op("pool", lambda e, rb=rb: e.tensor_copy(out=rtb, in_=rtl[rb][:, 0:256]), reads=[Trtl[rb]], writes=[Trtb])
                        for blk in range(128 // OB):
                            ob_ = obn % 2; obn += 1
                            t0b = blk * OB
                            oh1v = mk(OH1[ob_], [(128, OB), (1, 128)]); oh2v = mk(OH2[ob_], [(128, OB), (1, 128)])
                            for tl in range(OB):
                                tok = t0b + tl
                                P.op("dve", lambda e, tl=tl, tok=tok, ob_=ob_, rb=rb: e.tensor_scalar(
                                    out=OH1[ob_][:, tl * 128:(tl + 1) * 128], in0=iob128, scalar1=rtl[rb][:, tok:tok + 1], scalar2=rtl[rb][:, 256 + tok:257 + tok],
                                    op0=ALU.is_equal, op1=ALU.mult), reads=[Trtl[rb], Tio], writes=[TOH1[ob_]])
                            P.op("dve", lambda e, oh2v=oh2v, t0b=t0b: e.tensor_tensor(out=oh2v, in0=iobv, in1=mk(rtb, [(1, OB), (0, 128)], off=128 + t0b), op=ALU.is_equal),
                                 reads=[Trtb, Tio], writes=[TOH2[ob_]])
                            for t4 in range(OB // 4):
                                wb = 2 + (t4 % 2)
                                for q4 in range(4):
                                    tl = t4 * 4 + q4
                                    P.op("pe", lambda e, tl=tl, q4=q4, wb=wb, ob_=ob_: e.matmul(out=PS[wb][:, q4 * 128:(q4 + 1) * 128], lhsT=OH2[ob_][:, tl * 128:(tl + 1) * 128],
                                                                                            rhs=OH1[ob_][:, tl * 128:(tl + 1) * 128], start=True, stop=True),
                                         reads=[TOH1[ob_], TOH2[ob_]], writes=[PT_[wb]], inc=(q4 == 3))
                                atv = mk(AT, [(1, 4), (GT, 128)], off=tt * 128 + t0b + t4 * 4)
                                P.op("dve", lambda e, atv=atv, wb=wb: e.tensor_tensor(out=atv, in0=mk(PS[wb], [(128, 4), (1, 128)]), in1=atv, op=ALU.mult),
                                     reads=[PT_[wb]], writes=[TAT])
                    for i1 in range(128):
                        vb = i1 % NBUF
                        dq += 1
                        P.dma("sp" if dq % 2 else "act", lambda e, i1=i1, vb=vb: e.dma_start(out=Vc[vb], in_=Vs[i1]), reads=[th("VS", i1)], writes=[TVc[vb]])
                        for tt in range(nt_):
                            for hf2 in range(2):
                                bank = tt * 2 + hf2
                                P.op("pe", lambda e, i1=i1, vb=vb, tt=tt, hf2=hf2, bank=bank: e.matmul(
                                    out=PS[bank][:, :], lhsT=AT[:, i1 * GT + tt * 128:i1 * GT + (tt + 1) * 128], rhs=Vc[vb][:, hf2 * 512:(hf2 + 1) * 512],
                                    start=(i1 == 0), stop=(i1 == 127)), reads=[TAT, TVc[vb]], writes=[PT_[bank]], inc=(tt == nt_ - 1 and hf2 == 1))
                    for tt, tile in enumerate(tiles):
                        hb_ = tile % 2
                        i = tile
                        P.dma("sp", lambda e, tile=tile, hb_=hb_: e.dma_start(out=h2l[hb_], in_=H2s[s, tile * 128:(tile + 1) * 128, :]),
                              reads=[th("H2", s, tile)], writes=[Th2l[hb_]])
                        for hf2 in range(2):
                            bank = tt * 2 + hf2
                            P.op("dve", lambda e, hf2=hf2, bank=bank, hb_=hb_: e.tensor_tensor(out=po[:, hf2 * 512:(hf2 + 1) * 512], in0=PS[bank][:, :],
                                                                                              in1=h2l[hb_][:, hf2 * 512:(hf2 + 1) * 512], op=ALU.add),
                                 reads=[PT_[bank], Th2l[hb_]], writes=[Tpo])
                        if layer < DEPTH - 1:
                            P.dma("sp", lambda e, i=i: e.dma_start(out=H[s, i * 128:(i + 1) * 128, :], in_=po), reads=[Tpo], writes=[th("H", s, i)])
                        else:
                            P.op("dve", lambda e: e.scalar_tensor_tensor(out=junk, in0=po, scalar=1.0, in1=po, op0=ALU.mult, op1=ALU.mult, accum_out=st[:, 0:1]),
                                 reads=[Tpo], writes=[Tjunk, Tst])
                            rsqrt_(st[:, 2:3], st[:, 0:1], Tst, 1.0 / D)
                            P.op("dve", lambda e: e.scalar_tensor_tensor(out=yo, in0=po, scalar=st[:, 2:3], in1=gfb, op0=ALU.mult, op1=ALU.mult),
                                 reads=[Tpo, Tst, TW], writes=[Tyo])
                            if i == 0:
                                P.dma("sp", lambda e: e.dma_start(out=yout[s, 0:128 - NMETA, :], in_=yo[NMETA:128, :]), reads=[Tyo], writes=[th("Y", s, i)])
                            elif i < NT - 1:
                                P.dma("sp", lambda e, i=i: e.dma_start(out=yout[s, i * 128 - NMETA:(i + 1) * 128 - NMETA, :], in_=yo), reads=[Tyo], writes=[th("Y", s, i)])
                            else:
                                P.dma("sp", lambda e, i=i: e.dma_start(out=yout[s, i * 128 - NMETA:S, :], in_=yo[0:LR, :]), reads=[Tyo], writes=[th("Y", s, i)])
                P.barrier()
    except _Stop:
        pass
    P.barrier()

    with nc.Block() as block:
        @block.sync
        def _(e):
            P.replay("sp", e)

        @block.tensor
        def _(e):
            P.replay("pe", e)

        @block.vector
        def _(e):
            P.replay("dve", e)

        @block.scalar
        def _(e):
            P.replay("act", e)

        @block.gpsimd
        def _(e):
            P.replay("pool", e)
    es.close()
    return nc


def host_consts(S):
    L = S + NMETA
    NT = (L + 127) // 128
    LP = NT * 128
    N = 2 * LP - 2
    rows = S // 64
    row = np.repeat(np.arange(rows, dtype=np.float32), 64)
    colp = np.tile(np.arange(64, dtype=np.float32), rows)
    inv = (np.float32(10000.0) ** (-np.arange(16, dtype=np.float32) / np.float32(16))).astype(np.float32)
    ang = np.concatenate([row[:, None] * inv, colp[:, None] * inv], axis=-1).astype(np.float32)
    angp = np.zeros((LP, 32), np.float32)
    angp[NMETA:L] = ang
    c_cos = np.cos(angp).astype(np.float32)
    c_sin = np.sin(angp).astype(np.float32)
    a = np.arange(LP, dtype=np.int64)
    prod = (a[:, None] * a[None, :]) % N
    th_ = prod.astype(np.float64) * (2.0 * np.pi / N)
    Cm = np.cos(th_).astype(np.float32).astype(ml_dtypes.bfloat16)
    Sm = np.sin(th_).astype(np.float32).astype(ml_dtypes.bfloat16)
    del th_, prod

    def tile_(M):
        return np.ascontiguousarray(M.reshape(NT, 128, NT, 128).transpose(2, 1, 0, 3))

    c_C = tile_(Cm)
    c_S = tile_(Sm)
    t = np.arange(L, dtype=np.float32)
    tn = (t / np.float32(L - 1)).astype(np.float32)
    bands = np.linspace(1e-4, 15, 16, dtype=np.float32)
    w = (np.float32(2.0 * math.pi / L) * t).astype(np.float32)
    z = np.concatenate([tn[:, None], np.cos(w[:, None] * bands), np.sin(w[:, None] * bands)], axis=-1).astype(np.float32)
    zfT = np.zeros((33, LP), np.float32)
    zfT[:, :L] = z.T
    colc = np.zeros((128, 3, NT), np.float32)
    tnp = np.zeros(LP, np.float32); tnp[:L] = tn
    vp = np.zeros(LP, np.float32); vp[:L] = 1.0
    wf = np.full(LP, 2.0 / N, np.float32); wf[0] = 1.0 / N; wf[LP - 1] = 1.0 / N
    colc[:, 0, :] = (-tnp).reshape(NT, 128).T
    colc[:, 1, :] = vp.reshape(NT, 128).T
    colc[:, 2, :] = wf.reshape(NT, 128).T
    return dict(c_cos=c_cos, c_sin=c_sin, c_C=c_C, c_S=c_S, c_zfT=zfT, c_col=colc,
                c_idb=np.eye(128, dtype=np.float32).astype(ml_dtypes.bfloat16), c_idf=np.eye(128, dtype=np.float32),
                c_iota=np.tile(np.arange(16, dtype=np.float32)[None], (128, 1)),
                c_io128=np.tile(np.arange(128, dtype=np.float32)[None], (128, 1)).astype(ml_dtypes.bfloat16))


_WNAMES = ["meta_tokens", "norm1_g", "w_in", "q_norm_g", "k_norm_g", "hy_conv_w", "hy_conv_b", "hy_ffn_w1", "hy_ffn_b1",
           "hy_sin_freq", "hy_ffn_w2", "hy_ffn_b2", "hy_ffn_w3", "hy_decay", "hy_dskip", "attn_out_g", "hy_out_g", "w_out",
           "norm2_g", "peer_wq", "peer_keys", "peer_u", "peer_v", "final_g"]


def run(xs_per_core, weights, S, NSEQ, core_ids):
    nc = build(S, NSEQ)
    consts = host_consts(S)
    base = {k: np.ascontiguousarray(np.asarray(weights[k], dtype=np.float32)) for k in _WNAMES}
    base.update(consts)
    in_maps = []
    for xc in xs_per_core:
        m = dict(base)
        m["x"] = np.ascontiguousarray(xc, dtype=np.float32)
        in_maps.append(m)
    res = run_bass_kernel_spmd(nc, in_maps, core_ids=core_ids)
    return [r["y"] for r in res.results]


def kernel(**inputs):
    xp = np.asarray(inputs["x_prompt"], dtype=np.float32)
    xs = np.asarray(inputs["x_sample"], dtype=np.float32)
    S = xp.shape[1]
    per_core = []
    for c in range(8):
        per_core.append(np.concatenate([xp[2 * c:2 * c + 2], xs[c:c + 1]], axis=0))
    outs = run(per_core, inputs, S, 3, list(range(8)))
    yp = np.concatenate([o[0:2] for o in outs], axis=0)
    ys = np.concatenate([o[2:3] for o in outs], axis=0)
    return (yp.astype(np.float32), ys.astype(np.float32))
```

```python
import math
from contextlib import ExitStack
import numpy as np
import ml_dtypes
import concourse.bass as bass
import concourse.mybir as mybir
from concourse.bass_utils import run_bass_kernel_spmd

F32 = mybir.dt.float32
BF16 = mybir.dt.bfloat16
U8 = mybir.dt.uint8
U32 = mybir.dt.uint32
I32 = mybir.dt.int32
ALU = mybir.AluOpType
AF = mybir.ActivationFunctionType
AX = mybir.AxisListType

D = 1024
NMETA = 16
DEPTH = 2
NEXP = 16384
EPS = 1e-6
TWO_PI = 2.0 * math.pi


class Trk:
    __slots__ = ("w", "r")

    def __init__(self):
        self.w = None
        self.r = {}


class _Rec:
    def __getattr__(self, name):
        def f(*a, **k):
            return (name, a, k)
        return f


_REC = _Rec()


class Prog:
    ENG = ("pe", "dve", "act", "pool", "sp")

    def __init__(self, nc, es):
        self.nc = nc
        self.ops = {e: [] for e in self.ENG}
        self.sems = []
        self.val = []

        def newsem(name):
            h = es.enter_context(nc.semaphore(name))
            self.sems.append(h)
            self.val.append(0)
            return len(self.sems) - 1

        self.csem = {e: newsem("c_" + e) for e in ("pe", "dve", "act", "pool")}
        self.dsem = {"sp": [newsem("d_sp%d" % i) for i in range(12)],
                     "pool": [newsem("d_pl%d" % i) for i in range(8)],
                     "act": [newsem("d_ac%d" % i) for i in range(6)]}
        self.dn = {"sp": 0, "pool": 0, "act": 0}
        self.known = {e: {} for e in self.ENG}

    def _waits(self, eng, toks):
        kn = self.known[eng]
        for (s, v) in toks:
            if eng == "pe" and s == self.csem["pe"]:
                continue
            if kn.get(s, 0) < v:
                kn[s] = v
                self.ops[eng].append(("w", self.sems[s], v))

    @staticmethod
    def _deps(reads, writes):
        toks = []
        for t in reads:
            if t.w is not None:
                toks.append(t.w)
        for t in writes:
            if t.w is not None:
                toks.append(t.w)
            toks.extend(t.r.items())
        return toks

    @staticmethod
    def _mark(tok, reads, writes):
        for t in writes:
            t.w = tok
            t.r = {}
        for t in reads:
            if t in writes:
                continue
            if t.r.get(tok[0], 0) < tok[1]:
                t.r[tok[0]] = tok[1]

    def op(self, eng, fn, reads=(), writes=(), inc=True):
        self._waits(eng, self._deps(reads, writes))
        s = self.csem[eng]
        if inc:
            self.val[s] += 1
            tok = (s, self.val[s])
            self.ops[eng].append(("i", fn(_REC), self.sems[s], 1))
        else:
            tok = (s, self.val[s] + 1)
            self.ops[eng].append(("n", fn(_REC)))
        self._mark(tok, reads, writes)

    def dma(self, q, fn, reads=(), writes=()):
        toks = self._deps(reads, writes)
        pool = self.dsem[q]
        s = pool[self.dn[q] % len(pool)]
        self.dn[q] += 1
        if self.val[s] > 0:
            toks.append((s, self.val[s]))
        self._waits(q, toks)
        self.val[s] += 16
        tok = (s, self.val[s])
        self.ops[q].append(("i", fn(_REC), self.sems[s], 16))
        self._mark(tok, reads, writes)

    def barrier(self):
        allt = [(s, v) for s, v in enumerate(self.val) if v > 0]
        for e in self.ENG:
            self._waits(e, allt)

    def replay(self, eng, e):
        for it in self.ops[eng]:
            if it[0] == "w":
                e.wait_ge(it[1], it[2])
            elif it[0] == "i":
                c = it[1]
                getattr(e, c[0])(*c[1], **c[2]).then_inc(it[2], it[3])
            else:
                c = it[1]
                getattr(e, c[0])(*c[1], **c[2])


class _Stop(Exception):
    pass


import os
_KSTOP = os.environ.get("KSTOP", "")


def _chk(tag):
    if _KSTOP and _KSTOP == tag:
        raise _Stop()


def mk(base, dims, parts=None, p0=0, off=0):
    ps = list(base.ap[0])
    np_ = ps[1] if parts is None else parts
    return bass.AP(tensor=base.tensor, offset=base.offset + p0 * ps[0] + off,
                   ap=[[ps[0], np_]] + [[int(s), int(c)] for s, c in dims])


def build(S, NSEQ):
    L = S + NMETA
    NT = (L + 127) // 128
    LP = NT * 128
    LR = L - (NT - 1) * 128
    assert LP > L
    nc = bass.Bass("TRN2", target_bir_lowering=False)
    es = ExitStack()

    def din(name, shape, dt=F32):
        return nc.dram_tensor(name, list(shape), dt, kind="ExternalInput").ap()

    def dscr(name, shape, dt=F32):
        return nc.dram_tensor(name, list(shape), dt, kind="Internal").ap()

    x = din("x", [NSEQ, S, D])
    meta = din("meta_tokens", [NMETA, D])
    norm1_g = din("norm1_g", [DEPTH, D]); w_in = din("w_in", [DEPTH, D, 2304])
    q_norm_g = din("q_norm_g", [DEPTH, 64]); k_norm_g = din("k_norm_g", [DEPTH, 64])
    hy_conv_w = din("hy_conv_w", [DEPTH, 3, 1536]); hy_conv_b = din("hy_conv_b", [DEPTH, 1536])
    hy_ffn_w1 = din("hy_ffn_w1", [DEPTH, 33, 64]); hy_ffn_b1 = din("hy_ffn_b1", [DEPTH, 64])
    hy_sin_freq = din("hy_sin_freq", [DEPTH, 2, 64]); hy_ffn_w2 = din("hy_ffn_w2", [DEPTH, 64, 64])
    hy_ffn_b2 = din("hy_ffn_b2", [DEPTH, 64]); hy_ffn_w3 = din("hy_ffn_w3", [DEPTH, 64, 1024])
    hy_decay = din("hy_decay", [DEPTH, 2, 512]); hy_dskip = din("hy_dskip", [DEPTH, 512])
    attn_out_g = din("attn_out_g", [DEPTH, 8, 64]); hy_out_g = din("hy_out_g", [DEPTH, 8, 64])
    w_out = din("w_out", [DEPTH, D, D]); norm2_g = din("norm2_g", [DEPTH, D])
    peer_wq = din("peer_wq", [DEPTH, D, 2048]); peer_keys = din("peer_keys", [DEPTH, 2, 8, 128, 128])
    peer_u = din("peer_u", [DEPTH, NEXP, D]); peer_v = din("peer_v", [DEPTH, NEXP, D])
    final_g = din("final_g", [D])
    c_cos = din("c_cos", [LP, 32]); c_sin = din("c_sin", [LP, 32])
    c_C = din("c_C", [NT, 128, NT, 128], BF16); c_S = din("c_S", [NT, 128, NT, 128], BF16)
    c_zfT = din("c_zfT", [33, LP]); c_col = din("c_col", [128, 3, NT])
    c_idb = din("c_idb", [128, 128], BF16); c_idf = din("c_idf", [128, 128])
    c_iota = din("c_iota", [128, 16])
    c_io128 = din("c_io128", [128, 128], BF16)
    yout = nc.dram_tensor("y", [NSEQ, S, D], F32, kind="ExternalOutput").ap()
    H = dscr("H", [NSEQ, LP, D])
    Zs = dscr("Zs", [NSEQ, LP + 2, 1536])
    X1s = dscr("X1s", [NSEQ, LP, 512])
    HRI = dscr("HRI", [NT, 128, 2, 512])
    MIXA = dscr("MIXA", [NSEQ, 64, 8, LP], BF16)
    MIXH = dscr("MIXH", [NSEQ, 128, 4, LP], BF16)
    H2s = dscr("H2s", [NSEQ, LP, D])
    HN2T = dscr("HN2T", [NSEQ, 128, 8, LP], BF16)
    RT = dscr("RT", [NSEQ, NT, 128, 384])
    UTs = dscr("UTs", [128, 128, 1024], BF16)
    Vs = dscr("Vs", [128, 128, 1024], BF16)

    ARENA = 200 * 1024
    arena = es.enter_context(nc.sbuf_tensor("arena", [128, ARENA], U8))
    psum = [es.enter_context(nc.psum_tensor("ps%d" % i, [128, 512], F32)) for i in range(8)]
    P = Prog(nc, es)

    class Alloc:
        def __init__(self, start):
            self.off = start

        def get(self, nelem, dt, parts=128):
            bs = {F32: 4, BF16: 2, U32: 4, U8: 1, I32: 4}[dt]
            nb = (nelem * bs + 31) // 32 * 32
            o = self.off
            self.off += nb
            assert self.off <= ARENA, ("arena overflow", self.off)
            return arena[0:parts, o:o + nelem * bs].bitcast(dt)

    PS = [p[:] for p in psum]
    PSB = [p[:].bitcast(BF16) for p in psum]
    PT_ = [Trk() for _ in range(8)]

    def col(j):
        return slice(j * 128, (j + 1) * 128)

    A0 = Alloc(0)
    idb = A0.get(128, BF16); idf = A0.get(128, F32)
    ccol = A0.get(3 * NT, F32)
    iota16 = A0.get(16, F32)
    negpi = A0.get(1, F32)
    epsc = A0.get(1, F32)
    Tc = Trk()
    P.dma("sp", lambda e: e.dma_start(out=idb, in_=c_idb), writes=[Tc])
    P.dma("sp", lambda e: e.dma_start(out=idf, in_=c_idf), writes=[Tc])
    P.dma("sp", lambda e: e.dma_start(out=ccol, in_=c_col.rearrange("p a n -> p (a n)")), writes=[Tc])
    P.dma("sp", lambda e: e.dma_start(out=iota16, in_=c_iota), writes=[Tc])
    P.op("dve", lambda e: e.memset(negpi, -math.pi), writes=[Tc])
    P.op("dve", lambda e: e.memset(epsc, EPS), writes=[Tc])

    def rsqrt_(dst, src, t, scale, use_eps=True, extra_reads=()):
        if use_eps:
            P.op("act", lambda e: e.activation(out=dst, in_=src, func=AF.Sqrt, scale=scale, bias=epsc[0:dst.shape[0], 0:1]),
                 reads=[t, Tc] + list(extra_reads), writes=[t])
        else:
            P.op("act", lambda e: e.activation(out=dst, in_=src, func=AF.Sqrt, scale=scale), reads=[t] + list(extra_reads), writes=[t])
        P.op("dve", lambda e: e.reciprocal(out=dst, in_=dst), reads=[t], writes=[t])
    negtn = lambda j: ccol[:, j:j + 1]
    valid = lambda j: ccol[:, NT + j:NT + j + 1]
    wfc = lambda j: ccol[:, 2 * NT + j:2 * NT + j + 1]
    COMMON_END = A0.off
    P.barrier()

    TH = {}

    def th(*key):
        if key not in TH:
            TH[key] = Trk()
        return TH[key]

    def load_h(layer, s, i, dst, tdst, q="sp"):
        if layer > 0:
            P.dma(q, lambda e: e.dma_start(out=dst, in_=H[s, i * 128:(i + 1) * 128, :]),
                  reads=[th("H", s, i)], writes=[tdst])
            return
        if i == 0:
            P.dma(q, lambda e: e.dma_start(out=dst[0:NMETA, :], in_=meta), writes=[tdst])
            P.dma(q, lambda e: e.dma_start(out=dst[NMETA:128, :], in_=x[s, 0:128 - NMETA, :]), writes=[tdst])
        elif i < NT - 1:
            P.dma(q, lambda e: e.dma_start(out=dst, in_=x[s, i * 128 - NMETA:i * 128 + 128 - NMETA, :]), writes=[tdst])
        else:
            P.op("dve", lambda e: e.memset(dst, 0.0), writes=[tdst])
            P.dma(q, lambda e: e.dma_start(out=dst[0:LR, :], in_=x[s, i * 128 - NMETA:S, :]), writes=[tdst])

    def rmsnorm_bf(src, tsrc, gb, tgb, out_bf, tout, junk, tjunk, st, tst, j, out_f32=None):
        P.op("dve", lambda e: e.scalar_tensor_tensor(out=junk, in0=src, scalar=1.0, in1=src,
                                                     op0=ALU.mult, op1=ALU.mult, accum_out=st[:, 0:1]),
             reads=[tsrc], writes=[tjunk, tst])
        rsqrt_(st[:, 1:2], st[:, 0:1], tst, 1.0 / D)
        P.op("dve", lambda e: e.tensor_scalar(out=st[:, 2:3], in0=st[:, 1:2], scalar1=valid(j), scalar2=None,
                                              op0=ALU.mult), reads=[tst, Tc], writes=[tst])
        if out_f32 is None:
            P.op("dve", lambda e: e.scalar_tensor_tensor(out=out_bf, in0=src, scalar=st[:, 2:3], in1=gb,
                                                         op0=ALU.mult, op1=ALU.mult),
                 reads=[tsrc, tst, tgb], writes=[tout])
        else:
            P.op("dve", lambda e: e.scalar_tensor_tensor(out=out_f32[0], in0=src, scalar=st[:, 2:3], in1=gb,
                                                         op0=ALU.mult, op1=ALU.mult),
                 reads=[tsrc, tst, tgb], writes=[out_f32[1]])
            P.op("pool", lambda e: e.tensor_copy(out=out_bf, in_=out_f32[0]), reads=[out_f32[1]], writes=[tout])

    def transpose_to(src_bf, tsrc, nchunk, dst, tdst, bank, kparts=128, csz=128, eng="act"):
        for c in range(nchunk):
            P.op("pe", lambda e, c=c: e.transpose(out=PSB[bank][0:csz, c * 128:(c + 1) * 128],
                                                  in_=src_bf[:, c * csz:(c + 1) * csz], identity=idb),
                 reads=[tsrc, Tc], writes=[PT_[bank]])
        srcv = mk(PSB[bank], [(128, nchunk), (1, 128)], parts=csz)
        if eng == "act":
            P.op("act", lambda e: e.copy(out=dst, in_=srcv), reads=[PT_[bank]], writes=[tdst])
        else:
            P.op("dve", lambda e: e.tensor_copy(out=dst, in_=srcv), reads=[PT_[bank]], writes=[tdst])

    try:
        for layer in range(DEPTH):
            A = Alloc(COMMON_END)
            w1 = A.get(64, F32, 33); w2 = A.get(64, F32, 64); w3 = A.get(1024, F32, 64)
            cols = A.get(4, F32, 64)
            zfT = A.get(LP, F32, 33)
            decb = A.get(1024, F32); dsk = A.get(512, F32, 1)
            hp = A.get(NT * 512, BF16); hm = A.get(NT * 512, BF16)
            a1 = A.get(512, F32, 64); h1T = A.get(512, F32, 64); h2T = A.get(512, F32, 64)
            dec = A.get(1024, F32); hf_t = A.get(512, F32); hb_t = A.get(512, F32)
            CSt = [[A.get(NT * 128, BF16) for _ in range(2)] for _ in range(2)]
            hri = [A.get(1024, F32) for _ in range(2)]
            Tw = Trk(); Ta1 = Trk(); Th1 = Trk(); Th2 = Trk(); Tdec = Trk(); Thf = Trk(); Thp = Trk()
            Tcs = [[Trk(), Trk()], [Trk(), Trk()]]; Thri = [Trk(), Trk()]
            P.dma("sp", lambda e: e.dma_start(out=w1, in_=hy_ffn_w1[layer]), writes=[Tw])
            P.dma("sp", lambda e: e.dma_start(out=w2, in_=hy_ffn_w2[layer]), writes=[Tw])
            P.dma("sp", lambda e: e.dma_start(out=w3, in_=hy_ffn_w3[layer]), writes=[Tw])
            P.dma("sp", lambda e: e.dma_start(out=cols[:, 0:1], in_=hy_ffn_b1[layer].rearrange("(p o) -> p o", o=1)), writes=[Tw])
            P.dma("sp", lambda e: e.dma_start(out=cols[:, 1:2], in_=hy_sin_freq[layer, 0].rearrange("(p o) -> p o", o=1)), writes=[Tw])
            P.dma("sp", lambda e: e.dma_start(out=cols[:, 2:3], in_=hy_ffn_b2[layer].rearrange("(p o) -> p o", o=1)), writes=[Tw])
            P.dma("sp", lambda e: e.dma_start(out=cols[:, 3:4], in_=hy_sin_freq[layer, 1].rearrange("(p o) -> p o", o=1)), writes=[Tw])
            P.dma("sp", lambda e: e.dma_start(out=zfT, in_=c_zfT), writes=[Tw])
            P.dma("sp", lambda e: e.dma_start(out=decb, in_=hy_decay[layer].rearrange("a c -> (a c)").partition_broadcast(128)), writes=[Tw])
            P.dma("sp", lambda e: e.dma_start(out=dsk, in_=hy_dskip[layer].rearrange("(o c) -> o c", o=1)), writes=[Tw])
            ki_t = A.get(512, I32, 64); kf_t = A.get(512, F32, 64)

            def sin_rr(arg, dst, tdst):
                w_ = arg.shape[1]
                P.op("dve", lambda e: e.tensor_scalar(out=arg, in0=arg, scalar1=1.0 / TWO_PI, scalar2=64.0, op0=ALU.mult, op1=ALU.add), reads=[Ta1], writes=[Ta1])
                P.op("dve", lambda e: e.tensor_copy(out=ki_t[:, 0:w_], in_=arg), reads=[Ta1], writes=[Ta1])
                P.op("dve", lambda e: e.tensor_copy(out=kf_t[:, 0:w_], in_=ki_t[:, 0:w_]), reads=[Ta1], writes=[Ta1])
                P.op("dve", lambda e: e.tensor_tensor(out=arg, in0=arg, in1=kf_t[:, 0:w_], op=ALU.subtract), reads=[Ta1], writes=[Ta1])
                P.op("dve", lambda e: e.tensor_scalar(out=arg, in0=arg, scalar1=0.499999, scalar2=-0.499999, op0=ALU.min, op1=ALU.max), reads=[Ta1], writes=[Ta1])
                P.op("act", lambda e: e.activation(out=dst, in_=arg, func=AF.Sin, scale=TWO_PI), reads=[Ta1], writes=[tdst])
            for gidx in range((NT + 3) // 4):
                j0 = gidx * 4
                nj = min(4, NT - j0)
                w = nj * 128
                cs = slice(j0 * 128, j0 * 128 + w)
                P.op("pe", lambda e: e.matmul(out=PS[0][0:64, 0:w], lhsT=w1[0:33, 0:64], rhs=zfT[0:33, cs], start=True, stop=True),
                     reads=[Tw], writes=[PT_[0]])
                P.op("dve", lambda e: e.tensor_scalar(out=a1[:, 0:w], in0=PS[0][0:64, 0:w], scalar1=cols[:, 0:1], scalar2=cols[:, 1:2],
                                                      op0=ALU.add, op1=ALU.mult), reads=[PT_[0], Tw], writes=[Ta1])
                sin_rr(a1[:, 0:w], h1T[:, 0:w], Th1)
                P.op("pe", lambda e: e.matmul(out=PS[1][0:64, 0:w], lhsT=w2[0:64, 0:64], rhs=h1T[0:64, 0:w], start=True, stop=True),
                     reads=[Tw, Th1], writes=[PT_[1]])
                P.op("dve", lambda e: e.tensor_scalar(out=a1[:, 0:w], in0=PS[1][0:64, 0:w], scalar1=cols[:, 2:3], scalar2=cols[:, 3:4],
                                                      op0=ALU.add, op1=ALU.mult), reads=[PT_[1], Tw], writes=[Ta1])
                sin_rr(a1[:, 0:w], h2T[:, 0:w], Th2)
                for jj in range(nj):
                    j = j0 + jj
                    P.op("pe", lambda e, jj=jj: e.matmul(out=PS[2][:, :], lhsT=h2T[0:64, col(jj)], rhs=w3[0:64, 0:512], start=True, stop=True),
                         reads=[Th2, Tw], writes=[PT_[2]])
                    P.op("pe", lambda e, jj=jj: e.matmul(out=PS[3][:, :], lhsT=h2T[0:64, col(jj)], rhs=w3[0:64, 512:1024], start=True, stop=True),
                         reads=[Th2, Tw], writes=[PT_[3]])
                    P.op("act", lambda e, j=j: e.activation(out=dec, in_=decb, func=AF.Exp, scale=negtn(j)), reads=[Tw, Tc], writes=[Tdec])
                    P.op("dve", lambda e, j=j: e.scalar_tensor_tensor(out=hf_t, in0=PS[2][:, :], scalar=valid(j), in1=dec[:, 0:512],
                                                                      op0=ALU.mult, op1=ALU.mult), reads=[PT_[2], Tdec, Tc], writes=[Thf])
                    P.op("dve", lambda e, j=j: e.scalar_tensor_tensor(out=hb_t, in0=PS[3][:, :], scalar=valid(j), in1=dec[:, 512:1024],
                                                                      op0=ALU.mult, op1=ALU.mult), reads=[PT_[3], Tdec, Tc], writes=[Thf])
                    if j == 0:
                        P.op("dve", lambda e: e.memset(hb_t[0:1, :], 0.0), writes=[Thf])
                        P.op("dve", lambda e: e.tensor_tensor(out=hf_t[0:1, :], in0=hf_t[0:1, :], in1=dsk[0:1, :], op=ALU.add),
                             reads=[Tw], writes=[Thf])
                    P.op("dve", lambda e, j=j: e.tensor_tensor(out=hp[:, j * 512:(j + 1) * 512], in0=hf_t, in1=hb_t, op=ALU.add),
                         reads=[Thf], writes=[Thp])
                    P.op("dve", lambda e, j=j: e.tensor_tensor(out=hm[:, j * 512:(j + 1) * 512], in0=hb_t, in1=hf_t, op=ALU.subtract),
                         reads=[Thf], writes=[Thp])
            for j in range(NT):
                b = j % 2
                P.dma("sp", lambda e, j=j, b=b: e.dma_start(out=CSt[b][0], in_=c_C[j].rearrange("p a n -> p (a n)")), writes=[Tcs[b][0]])
                P.dma("act", lambda e, j=j, b=b: e.dma_start(out=CSt[b][1], in_=c_S[j].rearrange("p a n -> p (a n)")), writes=[Tcs[b][1]])
                for a in range(NT):
                    P.op("pe", lambda e, a=a, b=b: e.matmul(out=PS[4 + b][:, :], lhsT=CSt[b][0][:, col(a)], rhs=hp[:, a * 512:(a + 1) * 512],
                                                            start=(a == 0), stop=(a == NT - 1)),
                         reads=[Tcs[b][0], Thp], writes=[PT_[4 + b]], inc=(a == NT - 1))
                for a in range(NT):
                    P.op("pe", lambda e, a=a, b=b: e.matmul(out=PS[6 + b][:, :], lhsT=CSt[b][1][:, col(a)], rhs=hm[:, a * 512:(a + 1) * 512],
                                                            start=(a == 0), stop=(a == NT - 1)),
                         reads=[Tcs[b][1], Thp], writes=[PT_[6 + b]], inc=(a == NT - 1))
                P.op("dve", lambda e, j=j, b=b: e.tensor_scalar(out=hri[b][:, 0:512], in0=PS[4 + b][:, :], scalar1=wfc(j), scalar2=None, op0=ALU.mult),
                     reads=[PT_[4 + b], Tc], writes=[Thri[b]])
                P.op("dve", lambda e, j=j, b=b: e.tensor_scalar(out=hri[b][:, 512:1024], in0=PS[6 + b][:, :], scalar1=wfc(j), scalar2=None, op0=ALU.mult),
                     reads=[PT_[6 + b], Tc], writes=[Thri[b]])
                P.dma("sp", lambda e, j=j, b=b: e.dma_start(out=HRI[j].rearrange("p a c -> p (a c)"), in_=hri[b]),
                      reads=[Thri[b]], writes=[th("HRI", j)])
            P.barrier()
            _chk("F")
            A = Alloc(COMMON_END)
            ur = [A.get(D, F32) for _ in range(2)]; vr = [A.get(D, BF16) for _ in range(2)]; utb = [A.get(D, BF16) for _ in range(2)]
            Tur = [Trk(), Trk()]; Tvr = [Trk(), Trk()]; Tutb = [Trk(), Trk()]
            for i1 in range(128):
                b = i1 % 2
                P.dma("sp", lambda e, i1=i1, b=b: e.dma_start(out=ur[b], in_=peer_u[layer, i1 * 128:(i1 + 1) * 128, :]), writes=[Tur[b]])
                P.dma("pool", lambda e, i1=i1, b=b: e.dma_start(out=vr[b], in_=peer_v[layer, i1 * 128:(i1 + 1) * 128, :]), writes=[Tvr[b]])
                P.dma("act", lambda e, i1=i1, b=b: e.dma_start(out=Vs[i1], in_=vr[b]), reads=[Tvr[b]], writes=[th("VS", i1)])
                for k in range(8):
                    bank = 2 * b + (k // 4)
                    P.op("pe", lambda e, k=k, b=b, bank=bank: e.transpose(out=PS[bank][:, (k % 4) * 128:(k % 4 + 1) * 128], in_=ur[b][:, k * 128:(k + 1) * 128], identity=idf),
                         reads=[Tur[b], Tc], writes=[PT_[bank]])
                for hb2 in range(2):
                    P.op("act", lambda e, b=b, hb2=hb2: e.copy(out=utb[b][:, hb2 * 512:(hb2 + 1) * 512], in_=PS[2 * b + hb2][:, :]), reads=[PT_[2 * b + hb2]], writes=[Tutb[b]])
                P.dma("sp", lambda e, i1=i1, b=b: e.dma_start(out=UTs[i1], in_=utb[b]), reads=[Tutb[b]], writes=[th("UT", i1)])
            P.barrier()
            _chk("U")

            for s in range(NSEQ):
                A = Alloc(COMMON_END)
                Win = A.get(8 * 2304, BF16)
                QT = A.get(8 * LP, BF16, 64); KT = A.get(2 * LP, BF16, 64); VA = A.get(NT * 130, BF16)
                g1b = A.get(D, F32); gqk = A.get(640, F32)
                gat = A.get(8, F32, 64); anorm = A.get(64, F32, 65)
                ht = [A.get(D, F32) for _ in range(2)]; junk = A.get(D, F32); st = A.get(4, F32)
                hnb = A.get(D, BF16); hnT = A.get(D, BF16)
                qk = A.get(640, F32); sq = A.get(640, F32); ssq = A.get(16, F32); qn = A.get(640, F32)
                t1 = A.get(320, F32); t2 = A.get(320, F32); qr = A.get(640, BF16)
                cs_t = [A.get(64, F32) for _ in range(2)]
                zt = [A.get(1536, F32) for _ in range(2)]
                PTb = [A.get(512, BF16) for _ in range(3)]
                oT = [A.get(512, F32, 65) for _ in range(2)]; osq = A.get(512, F32, 65); rs = A.get(512, F32, 64)
                mo = [A.get(512, BF16, 64) for _ in range(2)]
                zrow = A.get(1536, F32, 1)
                TWin = Trk(); Tg = Trk(); Tht = [Trk(), Trk()]; Tjunk = Trk(); Tst = Trk(); Thnb = Trk(); ThnT = Trk()
                Tqk = Trk(); Tsq = Trk(); Tqn = Trk(); Tt = Trk(); Tqr = Trk(); Tcs_t = [Trk(), Trk()]; Tzt = [Trk(), Trk()]
                TQ = {}; TK = {}; TV = {}
                TPT = [Trk() for _ in range(3)]; ToT = [Trk(), Trk()]; Tosq = Trk(); Trs = Trk(); Tmo = [Trk(), Trk()]
                for k in range(8):
                    for hf_ in range(2):
                        P.dma("pool", lambda e, k=k, hf_=hf_: e.dma_start(out=Win[:, k * 2304 + hf_ * 1152:k * 2304 + (hf_ + 1) * 1152],
                                                                          in_=w_in[layer, k * 128:(k + 1) * 128, hf_ * 1152:(hf_ + 1) * 1152]), writes=[TWin])
                P.dma("sp", lambda e: e.dma_start(out=g1b, in_=norm1_g[layer].partition_broadcast(128)), writes=[Tg])
                for hh in range(8):
                    P.dma("sp", lambda e, hh=hh: e.dma_start(out=gqk[:, hh * 64:(hh + 1) * 64], in_=q_norm_g[layer].partition_broadcast(128)), writes=[Tg])
                for hh in range(8, 10):
                    P.dma("sp", lambda e, hh=hh: e.dma_start(out=gqk[:, hh * 64:(hh + 1) * 64], in_=k_norm_g[layer].partition_broadcast(128)), writes=[Tg])
                P.dma("sp", lambda e: e.dma_start(out=gat, in_=attn_out_g[layer].rearrange("h d -> d h"), allow_slow_non_contiguous=True), writes=[Tg])
                P.op("dve", lambda e: e.memset(anorm[0:64, :], 1.0 / 64), writes=[Tg])
                P.op("dve", lambda e: e.memset(anorm[64:65, :], EPS), writes=[Tg])
                P.op("dve", lambda e: e.memset(VA, 1.0), writes=[Tg])
                P.op("dve", lambda e: e.memset(zrow, 0.0), writes=[Tg])
                P.dma("sp", lambda e: e.dma_start(out=Zs[s, 0:1, :], in_=zrow), reads=[Tg], writes=[th("Z", s, -1)])
                P.dma("sp", lambda e: e.dma_start(out=Zs[s, LP + 1:LP + 2, :], in_=zrow), reads=[Tg], writes=[th("Z", s, NT)])
                load_h(layer, s, 0, ht[0], Tht[0])
                for i in range(NT):
                    b = i % 2
                    if i + 1 < NT:
                        load_h(layer, s, i + 1, ht[1 - b], Tht[1 - b])
                    P.dma("act", lambda e, i=i, b=b: e.dma_start(out=cs_t[b][:, 0:32], in_=c_cos[i * 128:(i + 1) * 128, :]), writes=[Tcs_t[b]])
                    P.dma("act", lambda e, i=i, b=b: e.dma_start(out=cs_t[b][:, 32:64], in_=c_sin[i * 128:(i + 1) * 128, :]), writes=[Tcs_t[b]])
                    rmsnorm_bf(ht[b], Tht[b], g1b, Tg, hnb, Thnb, junk, Tjunk, st, Tst, i)
                    transpose_to(hnb, Thnb, 8, mk(hnT, [(128, 8), (1, 128)]), ThnT, 0)
                    for (c0, w_, bank) in ((0, 512, 1), (512, 256, 2)):
                        for k in range(8):
                            P.op("pe", lambda e, k=k, c0=c0, w_=w_, bank=bank: e.matmul(
                                out=PS[bank][:, 0:w_], lhsT=hnT[:, col(k)], rhs=Win[:, k * 2304 + c0:k * 2304 + c0 + w_],
                                start=(k == 0), stop=(k == 7)), reads=[ThnT, TWin], writes=[PT_[bank]], inc=(k == 7))
                    P.op("act", lambda e: e.copy(out=qk[:, 0:512], in_=PS[1][:, :]), reads=[PT_[1]], writes=[Tqk])
                    P.op("act", lambda e: e.copy(out=qk[:, 512:640], in_=PS[2][:, 0:128]), reads=[PT_[2]], writes=[Tqk])
                    TV[i] = Trk()
                    P.op("act", lambda e, i=i: e.copy(out=mk(VA, [(65, 2), (1, 64)], off=i * 130), in_=mk(PS[2], [(64, 2), (1, 64)], off=128)),
                         reads=[PT_[2], Tg], writes=[TV[i]])
                    for zi in range(3):
                        bank = 3 + zi
                        for k in range(8):
                            P.op("pe", lambda e, k=k, zi=zi, bank=bank: e.matmul(
                                out=PS[bank][:, :], lhsT=hnT[:, col(k)], rhs=Win[:, k * 2304 + 768 + zi * 512:k * 2304 + 768 + (zi + 1) * 512],
                                start=(k == 0), stop=(k == 7)), reads=[ThnT, TWin], writes=[PT_[bank]], inc=(k == 7))
                        P.op("act", lambda e, zi=zi, bank=bank, b=b: e.copy(out=zt[b][:, zi * 512:(zi + 1) * 512], in_=PS[bank][:, :]),
                             reads=[PT_[bank]], writes=[Tzt[b]])
                    P.dma("sp", lambda e, i=i, b=b: e.dma_start(out=Zs[s, 1 + i * 128:1 + (i + 1) * 128, :], in_=zt[b]),
                          reads=[Tzt[b]], writes=[th("Z", s, i)])
                    P.op("dve", lambda e: e.tensor_tensor(out=sq, in0=qk, in1=qk, op=ALU.mult), reads=[Tqk], writes=[Tsq])
                    P.op("dve", lambda e: e.tensor_reduce(out=ssq[:, 0:10], in_=mk(sq, [(64, 10), (1, 64)]), axis=AX.X, op=ALU.add),
                         reads=[Tsq], writes=[Tst])
                    rsqrt_(ssq[:, 0:10], ssq[:, 0:10], Tst, 1.0 / 64)
                    P.op("dve", lambda e: e.tensor_tensor(out=mk(qn, [(64, 10), (1, 64)]), in0=mk(qk, [(64, 10), (1, 64)]),
                                                          in1=mk(ssq, [(1, 10), (0, 64)]), op=ALU.mult), reads=[Tqk, Tst], writes=[Tqn])
                    P.op("dve", lambda e: e.tensor_tensor(out=qn, in0=qn, in1=gqk, op=ALU.mult), reads=[Tg], writes=[Tqn])
                    x0 = mk(qn, [(64, 10), (2, 32)]); x1 = mk(qn, [(64, 10), (2, 32)], off=1)
                    cb = mk(cs_t[b], [(0, 10), (1, 32)]); sb_ = mk(cs_t[b], [(0, 10), (1, 32)], off=32)
                    o0 = mk(qr, [(64, 10), (2, 32)]); o1 = mk(qr, [(64, 10), (2, 32)], off=1)
                    t1v = mk(t1, [(32, 10), (1, 32)]); t2v = mk(t2, [(32, 10), (1, 32)])
                    P.op("dve", lambda e, x0=x0, cb=cb: e.tensor_tensor(out=t1v, in0=x0, in1=cb, op=ALU.mult), reads=[Tqn, Tcs_t[b]], writes=[Tt])
                    P.op("dve", lambda e, x1=x1, sb_=sb_: e.tensor_tensor(out=t2v, in0=x1, in1=sb_, op=ALU.mult), reads=[Tqn, Tcs_t[b]], writes=[Tt])
                    P.op("dve", lambda e, o0=o0: e.tensor_tensor(out=o0, in0=t1v, in1=t2v, op=ALU.subtract), reads=[Tt], writes=[Tqr])
                    P.op("dve", lambda e, x0=x0, sb_=sb_: e.tensor_tensor(out=t1v, in0=x0, in1=sb_, op=ALU.mult), reads=[Tqn, Tcs_t[b]], writes=[Tt])
                    P.op("dve", lambda e, x1=x1, cb=cb: e.tensor_tensor(out=t2v, in0=x1, in1=cb, op=ALU.mult), reads=[Tqn, Tcs_t[b]], writes=[Tt])
                    P.op("dve", lambda e, o1=o1: e.tensor_tensor(out=o1, in0=t1v, in1=t2v, op=ALU.add), reads=[Tt], writes=[Tqr])
                    TQ[i] = Trk(); TK[i] = Trk()
                    transpose_to(qr, Tqr, 8, mk(QT, [(LP, 8), (1, 128)], off=i * 128), TQ[i], 6, csz=64)
                    transpose_to(qr[:, 512:640], Tqr, 2, mk(KT, [(LP, 2), (1, 128)], off=i * 128), TK[i], 7, csz=64, eng="dve")
                _chk("A")
                it = 0
                for qb in range(NT):
                    for g in range(2):
                        ob = 6 + g
                        for sbk in range(NT):
                            ks = 128 if sbk < NT - 1 else LR
                            sb_bank = it % 3
                            pb = it % 3
                            it += 1
                            P.op("pe", lambda e, g=g, sbk=sbk, ks=ks, qb=qb, sb_bank=sb_bank: e.matmul(
                                out=PS[sb_bank][0:ks, :], lhsT=KT[0:64, g * LP + sbk * 128:g * LP + sbk * 128 + ks],
                                rhs=mk(QT, [(LP, 4), (1, 128)], off=4 * g * LP + qb * 128), start=True, stop=True),
                                reads=[TK[sbk], TQ[qb]], writes=[PT_[sb_bank]])
                            P.op("act", lambda e, ks=ks, sb_bank=sb_bank, pb=pb: e.activation(
                                out=PTb[pb][0:ks, :], in_=PS[sb_bank][0:ks, :], func=AF.Exp, scale=0.125),
                                reads=[PT_[sb_bank]], writes=[TPT[pb]])
                            P.op("pe", lambda e, g=g, sbk=sbk, ks=ks, pb=pb, ob=ob: e.matmul(
                                out=PS[ob][0:65, :], lhsT=VA[0:ks, sbk * 130 + g * 65:sbk * 130 + g * 65 + 65], rhs=PTb[pb][0:ks, :],
                                start=(sbk == 0), stop=(sbk == NT - 1)), reads=[TV[sbk], TPT[pb]], writes=[PT_[ob]], inc=True)
                        P.op("dve", lambda e, g=g, ob=ob: e.tensor_copy(out=oT[g], in_=PS[ob][0:65, :]), reads=[PT_[ob]], writes=[ToT[g]])
                        P.op("dve", lambda e, g=g: e.tensor_tensor(out=osq, in0=oT[g], in1=oT[g], op=ALU.mult), reads=[ToT[g]], writes=[Tosq])
                        P.op("pe", lambda e: e.matmul(out=PS[3][0:64, :], lhsT=anorm[0:65, 0:64], rhs=osq[0:65, :], start=True, stop=True),
                             reads=[Tg, Tosq], writes=[PT_[3]])
                        rsqrt_(rs, PS[3][0:64, :], Trs, 1.0, use_eps=False, extra_reads=[PT_[3]])
                        for jh in range(4):
                            hh = 4 * g + jh
                            P.op("dve", lambda e, g=g, jh=jh, hh=hh: e.scalar_tensor_tensor(
                                out=mo[g][:, col(jh)], in0=oT[g][0:64, col(jh)], scalar=gat[:, hh:hh + 1], in1=rs[:, col(jh)],
                                op0=ALU.mult, op1=ALU.mult), reads=[ToT[g], Trs, Tg], writes=[Tmo[g]])
                        P.dma("sp", lambda e, g=g, qb=qb: e.dma_start(out=MIXA[s, :, 4 * g:4 * g + 4, qb * 128:(qb + 1) * 128],
                                                                     in_=mk(mo[g], [(128, 4), (1, 128)])),
                              reads=[Tmo[g]], writes=[th("MIXA", s, qb)])
                P.barrier()
                _chk("B")

                A = Alloc(COMMON_END)
                u_sb = A.get(NT * 512, BF16)
                C2_START = A.off
                zm = A.get(1536, F32); z0 = A.get(1536, F32); zp = A.get(1536, F32); acc = A.get(1536, F32)
                wt = [A.get(1536, F32) for _ in range(4)]
                x1t = A.get(512, F32); uf = A.get(512, F32)
                Twt = Trk(); Tz3 = Trk(); Tacc = Trk(); Tx1 = Trk(); Tuf = Trk(); Tu = Trk()
                for r in range(3):
                    P.dma("sp", lambda e, r=r: e.dma_start(out=wt[r], in_=hy_conv_w[layer, r].partition_broadcast(128)), writes=[Twt])
                P.dma("sp", lambda e: e.dma_start(out=wt[3], in_=hy_conv_b[layer].partition_broadcast(128)), writes=[Twt])
                for i in range(NT):
                    r0 = 1 + i * 128
                    zdeps = [th("Z", s, i - 1), th("Z", s, i), th("Z", s, min(i + 1, NT))]
                    P.dma("sp", lambda e, r0=r0: e.dma_start(out=zm, in_=Zs[s, r0 - 1:r0 + 127, :]), reads=zdeps, writes=[Tz3])
                    P.dma("act", lambda e, r0=r0: e.dma_start(out=z0, in_=Zs[s, r0:r0 + 128, :]), reads=zdeps, writes=[Tz3])
                    P.dma("sp", lambda e, r0=r0: e.dma_start(out=zp, in_=Zs[s, r0 + 1:r0 + 129, :]), reads=zdeps, writes=[Tz3])
                    P.op("dve", lambda e: e.tensor_tensor(out=acc, in0=zm, in1=wt[0], op=ALU.mult), reads=[Tz3, Twt], writes=[Tacc])
                    P.op("dve", lambda e: e.tensor_tensor(out=z0, in0=z0, in1=wt[1], op=ALU.mult), reads=[Twt], writes=[Tz3])
                    P.op("dve", lambda e: e.tensor_tensor(out=zp, in0=zp, in1=wt[2], op=ALU.mult), reads=[Twt], writes=[Tz3])
                    P.op("dve", lambda e: e.tensor_tensor(out=acc, in0=acc, in1=z0, op=ALU.add), reads=[Tz3], writes=[Tacc])
                    P.op("dve", lambda e: e.tensor_tensor(out=acc, in0=acc, in1=zp, op=ALU.add), reads=[Tz3], writes=[Tacc])
                    P.op("dve", lambda e: e.tensor_tensor(out=acc, in0=acc, in1=wt[3], op=ALU.add), reads=[Twt], writes=[Tacc])
                    P.op("act", lambda e: e.copy(out=x1t, in_=acc[:, 512:1024]), reads=[Tacc], writes=[Tx1])
                    P.dma("sp", lambda e, i=i: e.dma_start(out=X1s[s, i * 128:(i + 1) * 128, :], in_=x1t), reads=[Tx1], writes=[th("X1", s, i)])
                    P.op("dve", lambda e, i=i: e.scalar_tensor_tensor(out=u_sb[:, i * 512:(i + 1) * 512], in0=acc[:, 1024:1536], scalar=valid(i),
                                                                      in1=acc[:, 0:512], op0=ALU.mult, op1=ALU.mult),
                         reads=[Tacc, Tc], writes=[Tu])
                P.barrier()
                _chk("C1")
                A = Alloc(C2_START)
                Y = A.get(NT * 1024, BF16)
                CSt = [[A.get(NT * 128, BF16) for _ in range(2)] for _ in range(2)]
                hri = [A.get(1024, F32) for _ in range(2)]
                ab = [A.get(1024, F32) for _ in range(2)]
                m1 = A.get(512, F32); m2 = A.get(512, F32)
                gyb = A.get(512, F32); x1l = [A.get(512, F32) for _ in range(2)]
                oy = A.get(512, F32); osq2 = A.get(512, F32); st8 = A.get(8, F32); ob16 = A.get(512, BF16)
                mh = [A.get(512, BF16) for _ in range(2)]
                Tcs = [[Trk(), Trk()], [Trk(), Trk()]]; Thri = [Trk(), Trk()]; Tab = [Trk(), Trk()]; Tm = Trk()
                TY = Trk(); Tgy = Trk(); Tx1l = [Trk(), Trk()]; Toy = Trk(); Tst8 = Trk(); Tob = Trk(); Tmh = [Trk(), Trk()]
                P.dma("sp", lambda e: e.dma_start(out=gyb, in_=hy_out_g[layer].rearrange("g d -> (g d)").partition_broadcast(128)), writes=[Tgy])
                for j in range(NT):
                    b = j % 2
                    P.dma("sp", lambda e, j=j, b=b: e.dma_start(out=CSt[b][0], in_=c_C[j].rearrange("p a n -> p (a n)")), writes=[Tcs[b][0]])
                    P.dma("act", lambda e, j=j, b=b: e.dma_start(out=CSt[b][1], in_=c_S[j].rearrange("p a n -> p (a n)")), writes=[Tcs[b][1]])
                    P.dma("sp", lambda e, j=j, b=b: e.dma_start(out=hri[b], in_=HRI[j].rearrange("p a c -> p (a c)")), reads=[th("HRI", j)], writes=[Thri[b]])
                    for cs_i in range(2):
                        bank = 2 * b + cs_i
                        for a in range(NT):
                            P.op("pe", lambda e, a=a, b=b, cs_i=cs_i, bank=bank: e.matmul(
                                out=PS[bank][:, :], lhsT=CSt[b][cs_i][:, col(a)], rhs=u_sb[:, a * 512:(a + 1) * 512],
                                start=(a == 0), stop=(a == NT - 1)), reads=[Tcs[b][cs_i], Tu], writes=[PT_[bank]], inc=(a == NT - 1))
                        P.op("act", lambda e, b=b, cs_i=cs_i, bank=bank: e.copy(out=ab[b][:, cs_i * 512:(cs_i + 1) * 512], in_=PS[bank][:, :]),
                             reads=[PT_[bank]], writes=[Tab[b]])
                    Av = ab[b][:, 0:512]; Bv = ab[b][:, 512:1024]; Hr = hri[b][:, 0:512]; Hi = hri[b][:, 512:1024]
                    P.op("dve", lambda e, Av=Av, Hr=Hr: e.tensor_tensor(out=m1, in0=Av, in1=Hr, op=ALU.mult), reads=[Tab[b], Thri[b]], writes=[Tm])
                    P.op("pool", lambda e, Bv=Bv, Hi=Hi: e.tensor_tensor(out=m2, in0=Bv, in1=Hi, op=ALU.mult), reads=[Tab[b], Thri[b]], writes=[Tm])
                    P.op("dve", lambda e, j=j: e.tensor_tensor(out=Y[:, j * 1024:j * 1024 + 512], in0=m1, in1=m2, op=ALU.add), reads=[Tm], writes=[TY])
                    P.op("dve", lambda e, Bv=Bv, Hr=Hr: e.tensor_tensor(out=m1, in0=Bv, in1=Hr, op=ALU.mult), reads=[Tab[b], Thri[b]], writes=[Tm])
                    P.op("pool", lambda e, Av=Av, Hi=Hi: e.tensor_tensor(out=m2, in0=Av, in1=Hi, op=ALU.mult), reads=[Tab[b], Thri[b]], writes=[Tm])
                    P.op("dve", lambda e, j=j: e.tensor_tensor(out=Y[:, j * 1024 + 512:(j + 1) * 1024], in0=m1, in1=m2, op=ALU.subtract), reads=[Tm], writes=[TY])
                for j in range(NT):
                    b = j % 2
                    P.dma("sp", lambda e, j=j, b=b: e.dma_start(out=CSt[b][0], in_=c_C[j].rearrange("p a n -> p (a n)")), writes=[Tcs[b][0]])
                    P.dma("act", lambda e, j=j, b=b: e.dma_start(out=CSt[b][1], in_=c_S[j].rearrange("p a n -> p (a n)")), writes=[Tcs[b][1]])
                    P.dma("sp", lambda e, j=j, b=b: e.dma_start(out=x1l[b], in_=X1s[s, j * 128:(j + 1) * 128, :]), reads=[th("X1", s, j)], writes=[Tx1l[b]])
                    bank = 4 + b
                    for a in range(NT):
                        P.op("pe", lambda e, a=a, b=b, bank=bank: e.matmul(out=PS[bank][:, :], lhsT=CSt[b][0][:, col(a)], rhs=Y[:, a * 1024:a * 1024 + 512],
                                                                           start=(a == 0), stop=False), reads=[Tcs[b][0], TY], writes=[PT_[bank]], inc=False)
                    for a in range(NT):
                        P.op("pe", lambda e, a=a, b=b, bank=bank: e.matmul(out=PS[bank][:, :], lhsT=CSt[b][1][:, col(a)], rhs=Y[:, a * 1024 + 512:(a + 1) * 1024],
                                                                           start=False, stop=(a == NT - 1)), reads=[Tcs[b][1], TY], writes=[PT_[bank]], inc=(a == NT - 1))
                    P.op("dve", lambda e, b=b, bank=bank: e.tensor_tensor(out=oy, in0=PS[bank][:, :], in1=x1l[b], op=ALU.mult), reads=[PT_[bank], Tx1l[b]], writes=[Toy])
                    P.op("pool", lambda e: e.tensor_tensor(out=osq2, in0=oy, in1=oy, op=ALU.mult), reads=[Toy], writes=[Tst8])
                    P.op("dve", lambda e: e.tensor_reduce(out=st8, in_=mk(osq2, [(64, 8), (1, 64)]), axis=AX.X, op=ALU.add), reads=[Tst8], writes=[Tst8])
                    rsqrt_(st8, st8, Tst8, 1.0 / 64)
                    P.op("dve", lambda e: e.tensor_tensor(out=mk(oy, [(64, 8), (1, 64)]), in0=mk(oy, [(64, 8), (1, 64)]), in1=mk(st8, [(1, 8), (0, 64)]), op=ALU.mult),
                         reads=[Tst8], writes=[Toy])
                    P.op("dve", lambda e: e.tensor_tensor(out=ob16, in0=oy, in1=gyb, op=ALU.mult), reads=[Toy, Tgy], writes=[Tob])
                    transpose_to(ob16, Tob, 4, mk(mh[b], [(128, 4), (1, 128)]), Tmh[b], 6 + b, eng="act")
                    P.dma("sp", lambda e, j=j, b=b: e.dma_start(out=MIXH[s, :, :, j * 128:(j + 1) * 128], in_=mk(mh[b], [(128, 4), (1, 128)])),
                          reads=[Tmh[b]], writes=[th("MIXH", s, j)])
                P.barrier()
                _chk("C2")

                A = Alloc(COMMON_END)
                Woa = A.get(8 * 1024, BF16, 64); Woh = A.get(4 * 1024, BF16)
                wq = A.get(8 * 2048, BF16); keysT = A.get(16 * 128, BF16); kst = A.get(16 * 128, F32)
                g2b = A.get(D, F32); gfb = A.get(D, F32)
                ht = [A.get(D, F32) for _ in range(2)]; h2 = A.get(D, F32); junk = A.get(D, F32); st = A.get(4, F32)
                hn2b = A.get(D, BF16); hn2f = A.get(D, F32); hn2T = A.get(D, BF16)
                mat = [A.get(8 * 128, BF16, 64) for _ in range(2)]; mht = [A.get(4 * 128, BF16) for _ in range(2)]
                qT = A.get(16 * 128, BF16)
                s_sb = A.get(2048, F32); s2_sb = A.get(2048, F32)
                sv = A.get(256, F32); si = A.get(256, U32); sif = A.get(256, F32)
                cand = A.get(2048, F32); cand2 = A.get(2048, F32)
                tv = A.get(128, F32); tj = A.get(128, U32); tjf = A.get(128, F32); k1f = A.get(128, F32); k2f = A.get(128, F32); k1i = A.get(128, I32)
                eq = A.get(2048, F32); i1s = A.get(128, F32); i2s = A.get(128, F32); eidf = A.get(128, F32); eid = A.get(128, U32)
                nm = A.get(8, F32); Zs_ = A.get(8, F32); gE = A.get(128, F32); av = A.get(128, F32); wv = A.get(128, F32)
                rtT = A.get(384, F32); TrtT = Trk()
                TW = Trk(); Tht = [Trk(), Trk()]; Th2 = Trk(); Tjunk = Trk(); Tst = Trk(); Thn = Trk(); ThnT = Trk(); Thnf = Trk()
                Tmat = [Trk(), Trk()]; Tmht = [Trk(), Trk()]; TqT = Trk(); Ts = Trk(); Ts2 = Trk(); Tsv = Trk(); Tcand = Trk()
                Ttv = Trk(); Tk = Trk(); Teq = Trk(); Teid = Trk(); Tgw = Trk(); Tav = Trk()
                Tpacc = Trk(); Tyo = Trk(); Tkst = Trk()
                for hh in range(8):
                    P.dma("pool", lambda e, hh=hh: e.dma_start(out=Woa[:, hh * 1024:(hh + 1) * 1024], in_=w_out[layer, hh * 64:(hh + 1) * 64, :]), writes=[TW])
                for c in range(4):
                    P.dma("pool", lambda e, c=c: e.dma_start(out=Woh[:, c * 1024:(c + 1) * 1024], in_=w_out[layer, 512 + c * 128:512 + (c + 1) * 128, :]), writes=[TW])
                for k in range(8):
                    P.dma("pool", lambda e, k=k: e.dma_start(out=wq[:, k * 2048:(k + 1) * 2048], in_=peer_wq[layer, k * 128:(k + 1) * 128, :]), writes=[TW])
                P.dma("sp", lambda e: e.dma_start(out=g2b, in_=norm2_g[layer].partition_broadcast(128)), writes=[TW])
                P.dma("sp", lambda e: e.dma_start(out=gfb, in_=final_g.partition_broadcast(128)), writes=[TW])
                for hh in range(8):
                    for pp in range(2):
                        ci = 2 * hh + pp
                        P.dma("sp", lambda e, hh=hh, pp=pp, ci=ci: e.dma_start(out=kst[:, ci * 128:(ci + 1) * 128], in_=peer_keys[layer, pp, hh]), writes=[Tkst])
                for ci in range(16):
                    bank = ci % 2
                    P.op("pe", lambda e, ci=ci, bank=bank: e.transpose(out=PS[bank][:, 0:128], in_=kst[:, ci * 128:(ci + 1) * 128], identity=idf),
                         reads=[Tkst, Tc], writes=[PT_[bank]])
                    P.op("act", lambda e, ci=ci, bank=bank: e.copy(out=keysT[:, ci * 128:(ci + 1) * 128], in_=PS[bank][:, 0:128]), reads=[PT_[bank]], writes=[TW])
                load_h(layer, s, 0, ht[0], Tht[0])
                for i in range(NT):
                    b = i % 2
                    if i + 1 < NT:
                        load_h(layer, s, i + 1, ht[1 - b], Tht[1 - b])
                    P.dma("act", lambda e, i=i, b=b: e.dma_start(out=mk(mat[b], [(128, 8), (1, 128)]), in_=MIXA[s, :, :, i * 128:(i + 1) * 128]),
                          reads=[th("MIXA", s, i)], writes=[Tmat[b]])
                    P.dma("act", lambda e, i=i, b=b: e.dma_start(out=mk(mht[b], [(128, 4), (1, 128)]), in_=MIXH[s, :, :, i * 128:(i + 1) * 128]),
                          reads=[th("MIXH", s, i)], writes=[Tmht[b]])
                    for cg in range(2):
                        bank = cg
                        for hh in range(8):
                            P.op("pe", lambda e, hh=hh, cg=cg, b=b, bank=bank: e.matmul(
                                out=PS[bank][:, :], lhsT=mat[b][0:64, col(hh)], rhs=Woa[0:64, hh * 1024 + cg * 512:hh * 1024 + (cg + 1) * 512],
                                start=(hh == 0), stop=False), reads=[Tmat[b], TW], writes=[PT_[bank]], inc=False)
                        for c in range(4):
                            P.op("pe", lambda e, c=c, cg=cg, b=b, bank=bank: e.matmul(
                                out=PS[bank][:, :], lhsT=mht[b][:, col(c)], rhs=Woh[:, c * 1024 + cg * 512:c * 1024 + (cg + 1) * 512],
                                start=False, stop=(c == 3)), reads=[Tmht[b], TW], writes=[PT_[bank]], inc=(c == 3))
                        P.op("dve", lambda e, cg=cg, b=b, bank=bank: e.tensor_tensor(out=h2[:, cg * 512:(cg + 1) * 512], in0=PS[bank][:, :],
                                                                                     in1=ht[b][:, cg * 512:(cg + 1) * 512], op=ALU.add),
                             reads=[PT_[bank], Tht[b]], writes=[Th2])
                    rmsnorm_bf(h2, Th2, g2b, TW, hn2b, Thn, junk, Tjunk, st, Tst, i, out_f32=(hn2f, Thnf))
                    transpose_to(hn2b, Thn, 8, mk(hn2T, [(128, 8), (1, 128)]), ThnT, 2)
                    for c4 in range(4):
                        bank = 3 + (c4 % 2)
                        for cc in range(4):
                            ci = c4 * 4 + cc
                            for k in range(8):
                                P.op("pe", lambda e, ci=ci, cc=cc, k=k, bank=bank: e.matmul(
                                    out=PS[bank][:, col(cc)], lhsT=wq[:, k * 2048 + ci * 128:k * 2048 + (ci + 1) * 128], rhs=hn2T[:, col(k)],
                                    start=(k == 0), stop=(k == 7)), reads=[TW, ThnT], writes=[PT_[bank]], inc=(k == 7 and cc == 3))
                        P.op("act", lambda e, c4=c4, bank=bank: e.copy(out=qT[:, c4 * 512:(c4 + 1) * 512], in_=PS[bank][:, :]), reads=[PT_[bank]], writes=[TqT])
                    for c4 in range(4):
                        bank = 5 + (c4 % 2)
                        for cc in range(4):
                            ci = c4 * 4 + cc
                            P.op("pe", lambda e, ci=ci, cc=cc, bank=bank: e.matmul(out=PS[bank][:, col(cc)], lhsT=qT[:, col(ci)], rhs=keysT[:, col(ci)],
                                                                                    start=True, stop=True), reads=[TqT, TW], writes=[PT_[bank]], inc=(cc == 3))
                        P.op("act", lambda e, c4=c4, bank=bank: e.copy(out=s_sb[:, c4 * 512:(c4 + 1) * 512], in_=PS[bank][:, :]), reads=[PT_[bank]], writes=[Ts])
                    for ci in range(16):
                        sl = s_sb[:, col(ci)]; sl2 = s2_sb[:, col(ci)]
                        P.op("dve", lambda e, ci=ci, sl=sl: e.max(out=sv[:, ci * 16:ci * 16 + 8], in_=sl), reads=[Ts], writes=[Tsv])
                        P.op("dve", lambda e, ci=ci, sl=sl: e.max_index(out=si[:, ci * 16:ci * 16 + 8], in_max=sv[:, ci * 16:ci * 16 + 8], in_values=sl), reads=[Ts], writes=[Tsv])
                        P.op("dve", lambda e, ci=ci, sl=sl, sl2=sl2: e.match_replace(out=sl2, in_to_replace=sv[:, ci * 16:ci * 16 + 8], in_values=sl, imm_value=-1e30),
                             reads=[Ts, Tsv], writes=[Ts2])
                        P.op("dve", lambda e, ci=ci, sl2=sl2: e.max(out=sv[:, ci * 16 + 8:ci * 16 + 16], in_=sl2), reads=[Ts2], writes=[Tsv])
                        P.op("dve", lambda e, ci=ci, sl2=sl2: e.max_index(out=si[:, ci * 16 + 8:ci * 16 + 16], in_max=sv[:, ci * 16 + 8:ci * 16 + 16], in_values=sl2),
                             reads=[Ts2], writes=[Tsv])
                    P.op("dve", lambda e: e.tensor_copy(out=sif, in_=si), reads=[Tsv], writes=[Tsv])
                    s1b = mk(sv, [(32, 8), (1, 16), (0, 16)]); s2b = mk(sv, [(32, 8), (0, 16), (1, 16)], off=16)
                    P.op("dve", lambda e, s1b=s1b, s2b=s2b: e.tensor_tensor(out=mk(cand, [(256, 8), (16, 16), (1, 16)]), in0=s1b, in1=s2b, op=ALU.add),
                         reads=[Tsv], writes=[Tcand])
                    for hh in range(8):
                        cl = cand[:, hh * 256:(hh + 1) * 256]; cl2 = cand2[:, hh * 256:(hh + 1) * 256]
                        P.op("dve", lambda e, hh=hh, cl=cl: e.max(out=tv[:, hh * 16:hh * 16 + 8], in_=cl), reads=[Tcand], writes=[Ttv])
                        P.op("dve", lambda e, hh=hh, cl=cl: e.max_index(out=tj[:, hh * 16:hh * 16 + 8], in_max=tv[:, hh * 16:hh * 16 + 8], in_values=cl), reads=[Tcand], writes=[Ttv])
                        P.op("dve", lambda e, hh=hh, cl=cl, cl2=cl2: e.match_replace(out=cl2, in_to_replace=tv[:, hh * 16:hh * 16 + 8], in_values=cl, imm_value=-1e30),
                             reads=[Tcand, Ttv], writes=[Ts2])
                        P.op("dve", lambda e, hh=hh, cl2=cl2: e.max(out=tv[:, hh * 16 + 8:hh * 16 + 16], in_=cl2), reads=[Ts2], writes=[Ttv])
                        P.op("dve", lambda e, hh=hh, cl2=cl2: e.max_index(out=tj[:, hh * 16 + 8:hh * 16 + 16], in_max=tv[:, hh * 16 + 8:hh * 16 + 16], in_values=cl2),
                             reads=[Ts2], writes=[Ttv])
                    P.op("dve", lambda e: e.tensor_copy(out=tjf, in_=tj), reads=[Ttv], writes=[Tk])
                    P.op("dve", lambda e: e.tensor_scalar(out=k1f, in0=tjf, scalar1=1.0 / 16, scalar2=-0.46875, op0=ALU.mult, op1=ALU.add), reads=[Tk], writes=[Tk])
                    P.op("dve", lambda e: e.tensor_copy(out=k1i, in_=k1f), reads=[Tk], writes=[Tk])
                    P.op("dve", lambda e: e.tensor_copy(out=k1f, in_=k1i), reads=[Tk], writes=[Tk])
                    P.op("dve", lambda e: e.scalar_tensor_tensor(out=k2f, in0=k1f, scalar=-16.0, in1=tjf, op0=ALU.mult, op1=ALU.add), reads=[Tk], writes=[Tk])
                    iob = mk(iota16, [(0, 128), (1, 16)])
                    for (kf, ioff, dst) in ((k1f, 0, i1s), (k2f, 16, i2s)):
                        P.op("dve", lambda e, kf=kf: e.tensor_tensor(out=mk(eq, [(16, 128), (1, 16)]), in0=iob, in1=mk(kf, [(1, 128), (0, 16)]), op=ALU.is_equal),
                             reads=[Tk, Tc], writes=[Teq])
                        P.op("dve", lambda e, ioff=ioff: e.tensor_tensor(out=mk(eq, [(256, 8), (16, 16), (1, 16)]), in0=mk(eq, [(256, 8), (16, 16), (1, 16)]),
                                                                         in1=mk(sif, [(32, 8), (0, 16), (1, 16)], off=ioff), op=ALU.mult), reads=[Tsv], writes=[Teq])
                        P.op("dve", lambda e, dst=dst: e.tensor_reduce(out=dst, in_=mk(eq, [(16, 128), (1, 16)]), axis=AX.X, op=ALU.add), reads=[Teq], writes=[Teid])
                    P.op("dve", lambda e: e.tensor_scalar(out=nm, in0=mk(tv, [(16, 8)]), scalar1=-1.0, scalar2=None, op0=ALU.mult), reads=[Ttv], writes=[Tgw])
                    for hh in range(8):
                        P.op("act", lambda e, hh=hh: e.activation(out=gE[:, hh * 16:(hh + 1) * 16], in_=tv[:, hh * 16:(hh + 1) * 16], func=AF.Exp,
                                                                  bias=nm[:, hh:hh + 1], scale=1.0), reads=[Ttv, Tgw], writes=[Tgw])
                    P.op("dve", lambda e: e.tensor_reduce(out=Zs_, in_=mk(gE, [(16, 8), (1, 16)]), axis=AX.X, op=ALU.add), reads=[Tgw], writes=[Tgw])
                    P.op("dve", lambda e: e.reciprocal(out=Zs_, in_=Zs_), reads=[Tgw], writes=[Tgw])
                    P.op("dve", lambda e: e.tensor_tensor(out=mk(gE, [(16, 8), (1, 16)]), in0=mk(gE, [(16, 8), (1, 16)]), in1=mk(Zs_, [(1, 8), (0, 16)]), op=ALU.mult),
                         reads=[Tgw], writes=[Tgw])
                    P.dma("sp", lambda e, i=i: e.dma_start(out=H2s[s, i * 128:(i + 1) * 128, :], in_=h2), reads=[Th2], writes=[th("H2", s, i)])
                    P.dma("act", lambda e, i=i: e.dma_start(out=HN2T[s, :, :, i * 128:(i + 1) * 128], in_=mk(hn2T, [(128, 8), (1, 128)])),
                          reads=[ThnT], writes=[th("HN2T", s, i)])
                    for c3, (src_, tsrc_) in enumerate(((i1s, Teid), (i2s, Teid), (gE, Tgw))):
                        P.op("pe", lambda e, c3=c3, src_=src_: e.transpose(out=PS[7][:, c3 * 128:(c3 + 1) * 128], in_=src_, identity=idf),
                             reads=[tsrc_, Tc], writes=[PT_[7]])
                    P.op("act", lambda e: e.copy(out=rtT, in_=PS[7][:, 0:384]), reads=[PT_[7]], writes=[TrtT])
                    P.dma("sp", lambda e, i=i: e.dma_start(out=RT[s, i], in_=rtT), reads=[TrtT], writes=[th("RT", s, i)])
                P.barrier()
                _chk("D1")
                A = Alloc(COMMON_END)
                GT = 512
                NBUF = 5
                OB = 16
                AT = A.get(128 * GT, BF16); hng = A.get(8 * GT, BF16)
                UTc = [A.get(1024, BF16) for _ in range(NBUF)]; Vc = [A.get(1024, BF16) for _ in range(NBUF)]
                rtl = [A.get(384, F32) for _ in range(2)]; rtb = A.get(256, BF16)
                OH1 = [A.get(OB * 128, BF16) for _ in range(2)]; OH2 = [A.get(OB * 128, BF16) for _ in range(2)]; iob128 = A.get(128, BF16)
                gfb = A.get(D, F32); h2l = [A.get(D, F32) for _ in range(2)]; po = A.get(D, F32); st = A.get(4, F32); yo = A.get(D, F32); junk = yo
                TAT = Trk(); Thng = Trk(); TUT = [Trk() for _ in range(NBUF)]; TVc = [Trk() for _ in range(NBUF)]; Trtl = [Trk(), Trk()]; Trtb = Trk()
                TOH1 = [Trk(), Trk()]; TOH2 = [Trk(), Trk()]; Tio = Trk(); TW = Trk(); Th2l = [Trk(), Trk()]; Tpo = Trk(); Tjunk = Trk(); Tst = Trk(); Tyo = Trk()
                P.dma("sp", lambda e: e.dma_start(out=gfb, in_=final_g.partition_broadcast(128)), writes=[TW])
                P.dma("sp", lambda e: e.dma_start(out=iob128, in_=c_io128), writes=[Tio])
                Tjunk = Tyo
                iobv = mk(iob128, [(0, OB), (1, 128)])
                groups = [list(range(t0_, min(t0_ + 4, NT))) for t0_ in range(0, NT, 4)]
                obn = 0
                dq = 0
                for tiles in groups:
                    nt_ = len(tiles); W_ = nt_ * 128; tok0 = tiles[0] * 128
                    P.dma("sp", lambda e, W_=W_, tok0=tok0: e.dma_start(out=mk(hng, [(GT, 8), (1, W_)]), in_=HN2T[s, :, :, tok0:tok0 + W_]),
                          reads=[th("HN2T", s, t_) for t_ in tiles], writes=[Thng])
                    for i1 in range(128):
                        ub = i1 % NBUF; bk = i1 % 2
                        dq += 1
                        P.dma("sp" if dq % 2 else "act", lambda e, i1=i1, ub=ub: e.dma_start(out=UTc[ub], in_=UTs[i1]), reads=[th("UT", i1)], writes=[TUT[ub]])
                        for k in range(8):
                            P.op("pe", lambda e, k=k, ub=ub, bk=bk, W_=W_: e.matmul(out=PS[bk][:, 0:W_], lhsT=UTc[ub][:, k * 128:(k + 1) * 128],
                                                                                    rhs=hng[:, k * GT:k * GT + W_], start=(k == 0), stop=(k == 7)),
                                 reads=[TUT[ub], Thng], writes=[PT_[bk]], inc=(k == 7))
                        P.op("act", lambda e, i1=i1, bk=bk, W_=W_: e.activation(out=AT[:, i1 * GT:i1 * GT + W_], in_=PS[bk][:, 0:W_], func=AF.Gelu),
                             reads=[PT_[bk]], writes=[TAT])
                    for tt, tile in enumerate(tiles):
                        rb = tile % 2
                        P.dma("sp", lambda e, tile=tile, rb=rb: e.dma_start(out=rtl[rb], in_=RT[s, tile]), reads=[th("RT", s, tile)], writes=[Trtl[rb]])
                        P.op("pool", lambda e, rb=rb: e.tensor_copy(out=rtb, in_=rtl[rb][:, 0:256]), reads=[Trtl[rb]], writes=[Trtb])
                        for blk in range(128 // OB):
                            ob_ = obn % 2; obn += 1
                            t0b = blk * OB
                            oh1v = mk(OH1[ob_], [(128, OB), (1, 128)]); oh2v = mk(OH2[ob_], [(128, OB), (1, 128)])
                            for tl in range(OB):
                                tok = t0b + tl
                                P.op("dve", lambda e, tl=tl, tok=tok, ob_=ob_, rb=rb: e.tensor_scalar(
                                    out=OH1[ob_][:, tl * 128:(tl + 1) * 128], in0=iob128, scalar1=rtl[rb][:, tok:tok + 1], scalar2=rtl[rb][:, 256 + tok:257 + tok],
                                    op0=ALU.is_equal, op1=ALU.mult), reads=[Trtl[rb], Tio], writes=[TOH1[ob_]])
                            P.op("dve", lambda e, oh2v=oh2v, t0b=t0b: e.tensor_tensor(out=oh2v, in0=iobv, in1=mk(rtb, [(1, OB), (0, 128)], off=128 + t0b), op=ALU.is_equal),
                                 reads=[Trtb, Tio], writes=[TOH2[ob_]])
                            for t4 in range(OB // 4):
                                wb = 2 + (t4 % 2)
                                for q4 in range(4):
                                    tl = t4 * 4 + q4
                                    P.op("pe", lambda e, tl=tl, q4=q4, wb=wb, ob_=ob_: e.matmul(out=mk(PS[wb], [(4, 128)], off=q4), lhsT=OH2[ob_][:, tl * 128:(tl + 1) * 128],
                                                                                            rhs=OH1[ob_][:, tl * 128:(tl + 1) * 128], start=True, stop=True),
                                         reads=[TOH1[ob_], TOH2[ob_]], writes=[PT_[wb]], inc=(q4 == 3))
                                atv = mk(AT, [(GT, 128), (1, 4)], off=tt * 128 + t0b + t4 * 4)
                                P.op("dve", lambda e, atv=atv, wb=wb: e.tensor_tensor(out=atv, in0=mk(PS[wb], [(4, 128), (1, 4)]), in1=atv, op=ALU.mult),
                                     reads=[PT_[wb]], writes=[TAT])
                    for i1 in range(128):
                        vb = i1 % NBUF
                        dq += 1
                        P.dma("sp" if dq % 2 else "act", lambda e, i1=i1, vb=vb: e.dma_start(out=Vc[vb], in_=Vs[i1]), reads=[th("VS", i1)], writes=[TVc[vb]])
                        for tt in range(nt_):
                            for hf2 in range(2):
                                bank = tt * 2 + hf2
                                P.op("pe", lambda e, i1=i1, vb=vb, tt=tt, hf2=hf2, bank=bank: e.matmul(
                                    out=PS[bank][:, :], lhsT=AT[:, i1 * GT + tt * 128:i1 * GT + (tt + 1) * 128], rhs=Vc[vb][:, hf2 * 512:(hf2 + 1) * 512],
                                    start=(i1 == 0), stop=(i1 == 127)), reads=[TAT, TVc[vb]], writes=[PT_[bank]], inc=(tt == nt_ - 1 and hf2 == 1))
                    for tt, tile in enumerate(tiles):
                        hb_ = tile % 2
                        i = tile
                        P.dma("sp", lambda e, tile=tile, hb_=hb_: e.dma_start(out=h2l[hb_], in_=H2s[s, tile * 128:(tile + 1) * 128, :]),
                              reads=[th("H2", s, tile)], writes=[Th2l[hb_]])
                        for hf2 in range(2):
                            bank = tt * 2 + hf2
                            P.op("dve", lambda e, hf2=hf2, bank=bank, hb_=hb_: e.tensor_tensor(out=po[:, hf2 * 512:(hf2 + 1) * 512], in0=PS[bank][:, :],
                                                                                              in1=h2l[hb_][:, hf2 * 512:(hf2 + 1) * 512], op=ALU.add),
                                 reads=[PT_[bank], Th2l[hb_]], writes=[Tpo])
                        if layer < DEPTH - 1:
                            P.dma("sp", lambda e, i=i: e.dma_start(out=H[s, i * 128:(i + 1) * 128, :], in_=po), reads=[Tpo], writes=[th("H", s, i)])
                        else:
                            P.op("dve", lambda e: e.scalar_tensor_tensor(out=junk, in0=po, scalar=1.0, in1=po, op0=ALU.mult, op1=ALU.mult, accum_out=st[:, 0:1]),
                                 reads=[Tpo], writes=[Tjunk, Tst])
                            rsqrt_(st[:, 2:3], st[:, 0:1], Tst, 1.0 / D)
                            P.op("dve", lambda e: e.scalar_tensor_tensor(out=yo, in0=po, scalar=st[:, 2:3], in1=gfb, op0=ALU.mult, op1=ALU.mult),
                                 reads=[Tpo, Tst, TW], writes=[Tyo])
                            if i == 0:
                                P.dma("sp", lambda e: e.dma_start(out=yout[s, 0:128 - NMETA, :], in_=yo[NMETA:128, :]), reads=[Tyo], writes=[th("Y", s, i)])
                            elif i < NT - 1:
                                P.dma("sp", lambda e, i=i: e.dma_start(out=yout[s, i * 128 - NMETA:(i + 1) * 128 - NMETA, :], in_=yo), reads=[Tyo], writes=[th("Y", s, i)])
                            else:
                                P.dma("sp", lambda e, i=i: e.dma_start(out=yout[s, i * 128 - NMETA:S, :], in_=yo[0:LR, :]), reads=[Tyo], writes=[th("Y", s, i)])
                P.barrier()
    except _Stop:
        pass
    P.barrier()

    with nc.Block() as block:
        @block.sync
        def _(e):
            P.replay("sp", e)

        @block.tensor
        def _(e):
            P.replay("pe", e)

        @block.vector
        def _(e):
            P.replay("dve", e)

        @block.scalar
        def _(e):
            P.replay("act", e)

        @block.gpsimd
        def _(e):
            P.replay("pool", e)
    es.close()
    return nc


def host_consts(S):
    L = S + NMETA
    NT = (L + 127) // 128
    LP = NT * 128
    N = 2 * LP - 2
    rows = S // 64
    row = np.repeat(np.arange(rows, dtype=np.float32), 64)
    colp = np.tile(np.arange(64, dtype=np.float32), rows)
    inv = (np.float32(10000.0) ** (-np.arange(16, dtype=np.float32) / np.float32(16))).astype(np.float32)
    ang = np.concatenate([row[:, None] * inv, colp[:, None] * inv], axis=-1).astype(np.float32)
    angp = np.zeros((LP, 32), np.float32)
    angp[NMETA:L] = ang
    c_cos = np.cos(angp).astype(np.float32)
    c_sin = np.sin(angp).astype(np.float32)
    a = np.arange(LP, dtype=np.int64)
    prod = (a[:, None] * a[None, :]) % N
    th_ = prod.astype(np.float64) * (2.0 * np.pi / N)
    Cm = np.cos(th_).astype(np.float32).astype(ml_dtypes.bfloat16)
    Sm = np.sin(th_).astype(np.float32).astype(ml_dtypes.bfloat16)
    del th_, prod

    def tile_(M):
        return np.ascontiguousarray(M.reshape(NT, 128, NT, 128).transpose(2, 1, 0, 3))

    c_C = tile_(Cm)
    c_S = tile_(Sm)
    t = np.arange(L, dtype=np.float32)
    tn = (t / np.float32(L - 1)).astype(np.float32)
    bands = np.linspace(1e-4, 15, 16, dtype=np.float32)
    w = (np.float32(2.0 * math.pi / L) * t).astype(np.float32)
    z = np.concatenate([tn[:, None], np.cos(w[:, None] * bands), np.sin(w[:, None] * bands)], axis=-1).astype(np.float32)
    zfT = np.zeros((33, LP), np.float32)
    zfT[:, :L] = z.T
    colc = np.zeros((128, 3, NT), np.float32)
    tnp = np.zeros(LP, np.float32); tnp[:L] = tn
    vp = np.zeros(LP, np.float32); vp[:L] = 1.0
    wf = np.full(LP, 2.0 / N, np.float32); wf[0] = 1.0 / N; wf[LP - 1] = 1.0 / N
    colc[:, 0, :] = (-tnp).reshape(NT, 128).T
    colc[:, 1, :] = vp.reshape(NT, 128).T
    colc[:, 2, :] = wf.reshape(NT, 128).T
    return dict(c_cos=c_cos, c_sin=c_sin, c_C=c_C, c_S=c_S, c_zfT=zfT, c_col=colc,
                c_idb=np.eye(128, dtype=np.float32).astype(ml_dtypes.bfloat16), c_idf=np.eye(128, dtype=np.float32),
                c_iota=np.tile(np.arange(16, dtype=np.float32)[None], (128, 1)),
                c_io128=np.tile(np.arange(128, dtype=np.float32)[None], (128, 1)).astype(ml_dtypes.bfloat16))


_WNAMES = ["meta_tokens", "norm1_g", "w_in", "q_norm_g", "k_norm_g", "hy_conv_w", "hy_conv_b", "hy_ffn_w1", "hy_ffn_b1",
           "hy_sin_freq", "hy_ffn_w2", "hy_ffn_b2", "hy_ffn_w3", "hy_decay", "hy_dskip", "attn_out_g", "hy_out_g", "w_out",
           "norm2_g", "peer_wq", "peer_keys", "peer_u", "peer_v", "final_g"]


def run(xs_per_core, weights, S, NSEQ, core_ids):
    nc = build(S, NSEQ)
    consts = host_consts(S)
    base = {k: np.ascontiguousarray(np.asarray(weights[k], dtype=np.float32)) for k in _WNAMES}
    base.update(consts)
    in_maps = []
    for xc in xs_per_core:
        m = dict(base)
        m["x"] = np.ascontiguousarray(xc, dtype=np.float32)
        in_maps.append(m)
    res = run_bass_kernel_spmd(nc, in_maps, core_ids=core_ids)
    return [r["y"] for r in res.results]


def kernel(**inputs):
    xp = np.asarray(inputs["x_prompt"], dtype=np.float32)
    xs = np.asarray(inputs["x_sample"], dtype=np.float32)
    S = xp.shape[1]
    per_core = []
    for c in range(8):
        per_core.append(np.concatenate([xp[2 * c:2 * c + 2], xs[c:c + 1]], axis=0))
    outs = run(per_core, inputs, S, 3, list(range(8)))
    yp = np.concatenate([o[0:2] for o in outs], axis=0)
    ys = np.concatenate([o[2:3] for o in outs], axis=0)
    return (yp.astype(np.float32), ys.astype(np.float32))
```
